# Optimizing a Trainium2 kernel written in Bass

```python
import jax, jax.numpy as jnp
from jax import lax
import numpy as np

D_MODEL = 1024
BATCH = 2
SEQ = 16384
DEPTH = 2

GRID_W = 64
CTX_LEN = 256
N_EVEN = (DEPTH + 1) // 2
N_ODD = DEPTH // 2
HEAD_DIM = 64
ATTN_HEADS = 8
KV_HEADS = 2
GQA_GROUP = ATTN_HEADS // KV_HEADS
WINDOW = 128
ATTN_BLOCK = 128
ROPE_BASE = 10000.0
GMLP_GROUPS = 8
GMLP_DIM = 64
CHUNK = 128
Q_W = ATTN_HEADS * HEAD_DIM
KV_W = KV_HEADS * HEAD_DIM
A_W = GMLP_GROUPS * GMLP_DIM
IN_W = Q_W + 2 * KV_W + 2 * A_W
MIX_W = Q_W + A_W
POOL_WINDOWS = (2, 4, 8, 16)
POOL_GROUPS = 4
POOL_DIM = D_MODEL // POOL_GROUPS
PEER_HEADS = 8
PEER_NKEYS = 128
PEER_EXPERTS = PEER_NKEYS * PEER_NKEYS
PEER_TOPK = 16
PEER_DKEY = 256
PEER_DHALF = PEER_DKEY // 2
PEER_BLOCK = 128
N_MOD = 6
EPS = 1e-6

kernel_name = 'hybrid_swa_gmlp_pool_peer_dit'


def rms_norm(x, g):
    xf = x.astype(jnp.float32)
    y = xf * lax.rsqrt(jnp.mean(jnp.square(xf), axis=-1, keepdims=True) + EPS)
    return (y * g).astype(x.dtype)


def axial_rope(t, row, col):
    half = HEAD_DIM // 2
    quarter = half // 2
    inv = jnp.power(ROPE_BASE, -jnp.arange(quarter, dtype=jnp.float32) / quarter)

    def rot(xa, pos):
        ang = pos[:, None] * inv[None, :]
        cos = jnp.cos(ang)[None, :, None, :]
        sin = jnp.sin(ang)[None, :, None, :]
        x1, x2 = xa[..., :quarter], xa[..., quarter:]
        return jnp.concatenate([x1 * cos - x2 * sin, x1 * sin + x2 * cos], axis=-1)

    tf = t.astype(jnp.float32)
    return jnp.concatenate([rot(tf[..., :half], row), rot(tf[..., half:], col)], axis=-1).astype(t.dtype)


def sink_softmax(logits, sink):
    sk = jnp.broadcast_to(sink.astype(jnp.float32).reshape(1, KV_HEADS, GQA_GROUP, 1, 1), logits.shape[:-1] + (1,))
    return jax.nn.softmax(jnp.concatenate([logits, sk], axis=-1), axis=-1)[..., :-1]


def window_attention(q, k, v, k_ctx, v_ctx, sink):
    B_, L_ = q.shape[0], q.shape[1]
    nb = L_ // ATTN_BLOCK
    span = ATTN_BLOCK + 2 * WINDOW
    scale = HEAD_DIM ** -0.5
    pad = ((0, 0), (WINDOW, WINDOW), (0, 0), (0, 0))
    kp = jnp.pad(k, pad)
    vp = jnp.pad(v, pad)
    qb = q.reshape(B_, nb, ATTN_BLOCK, KV_HEADS, GQA_GROUP, HEAD_DIM).swapaxes(0, 1)

    def one_block(args):
        n, qn = args
        start = n * ATTN_BLOCK
        kn = lax.dynamic_slice_in_dim(kp, start, span, axis=1)
        vn = lax.dynamic_slice_in_dim(vp, start, span, axis=1)
        qpos = start + jnp.arange(ATTN_BLOCK)
        kpos = start - WINDOW + jnp.arange(span)
        valid = (jnp.abs(qpos[:, None] - kpos[None, :]) <= WINDOW) & (kpos >= 0)[None, :] & (kpos < L_)[None, :]
        s_loc = jnp.einsum('bqhgd,bkhd->bhgqk', qn, kn).astype(jnp.float32) * scale
        s_loc = jnp.where(valid, s_loc, -jnp.inf)
        s_ctx = jnp.einsum('bqhgd,bkhd->bhgqk', qn, k_ctx).astype(jnp.float32) * scale
        p = sink_softmax(jnp.concatenate([s_loc, s_ctx], axis=-1), sink)
        o = (jnp.einsum('bhgqk,bkhd->bqhgd', p[..., :span], vn.astype(jnp.float32))
             + jnp.einsum('bhgqk,bkhd->bqhgd', p[..., span:], v_ctx.astype(jnp.float32)))
        return o.astype(q.dtype)

    out = lax.map(one_block, (jnp.arange(nb), qb))
    return out.swapaxes(0, 1).reshape(B_, L_, Q_W)


def context_attention(qc, kc, vc, sink):
    B_, C_ = qc.shape[0], qc.shape[1]
    qg = qc.reshape(B_, C_, KV_HEADS, GQA_GROUP, HEAD_DIM)
    s = jnp.einsum('bqhgd,bkhd->bhgqk', qg, kc).astype(jnp.float32) * (HEAD_DIM ** -0.5)
    p = sink_softmax(s, sink)
    o = jnp.einsum('bhgqk,bkhd->bqhgd', p, vc.astype(jnp.float32))
    return o.astype(qc.dtype).reshape(B_, C_, Q_W)


def chunk_gmlp(ua, va, w_s, b_s):
    B_, L_ = ua.shape[0], ua.shape[1]
    u = jax.nn.gelu(ua).reshape(B_, L_, GMLP_GROUPS, GMLP_DIM)
    vf = jax.nn.gelu(va).reshape(B_, L_, GMLP_GROUPS, GMLP_DIM).astype(jnp.float32)
    mu = jnp.mean(vf, axis=-1, keepdims=True)
    var = jnp.mean(jnp.square(vf - mu), axis=-1, keepdims=True)
    v = ((vf - mu) * lax.rsqrt(var + EPS)).astype(ua.dtype)
    vc = v.reshape(B_, L_ // CHUNK, CHUNK, GMLP_GROUPS, GMLP_DIM)
    mixed = jnp.einsum('gpq,bcqgd->bcpgd', w_s, vc) + b_s.T[:, :, None]
    return (u * mixed.reshape(B_, L_, GMLP_GROUPS, GMLP_DIM)).reshape(B_, L_, A_W)


def split_proj(p):
    return jnp.split(p, [Q_W, Q_W + KV_W, Q_W + 2 * KV_W, Q_W + 2 * KV_W + A_W], axis=-1)


def attn_gmlp_mixer(h, hc, w_in, w_out, sink, w_s, b_s, row, col, need_ctx_out):
    B_, L_ = h.shape[0], h.shape[1]
    C_ = hc.shape[1]
    q, k, v, ua, va = split_proj(h @ w_in)
    q = axial_rope(q.reshape(B_, L_, ATTN_HEADS, HEAD_DIM), row, col)
    k = axial_rope(k.reshape(B_, L_, KV_HEADS, HEAD_DIM), row, col)
    v = v.reshape(B_, L_, KV_HEADS, HEAD_DIM)
    if need_ctx_out:
        qc, kc, vc, uac, vac = split_proj(hc @ w_in)
    else:
        kc, vc = jnp.split(hc @ w_in[:, Q_W:Q_W + 2 * KV_W], 2, axis=-1)
    kc = kc.reshape(B_, C_, KV_HEADS, HEAD_DIM)
    vc = vc.reshape(B_, C_, KV_HEADS, HEAD_DIM)
    attn = window_attention(q, k, v, kc, vc, sink)
    mix = chunk_gmlp(ua, va, w_s, b_s)
    y = jnp.concatenate([attn, mix], axis=-1) @ w_out
    yc = None
    if need_ctx_out:
        attn_c = context_attention(qc.reshape(B_, C_, ATTN_HEADS, HEAD_DIM), kc, vc, sink)
        mix_c = chunk_gmlp(uac, vac, w_s, b_s)
        yc = jnp.concatenate([attn_c, mix_c], axis=-1) @ w_out
    return y, yc


def multiscale_pool(h, w_pool, scale):
    B_, L_ = h.shape[0], h.shape[1]
    hf = h.reshape(B_, L_, POOL_GROUPS, POOL_DIM).astype(jnp.float32)
    cs = jnp.pad(lax.cumsum(hf, axis=1), ((0, 0), (1, 0), (0, 0), (0, 0)))
    t = jnp.arange(L_)
    diffs = []
    for g, w in enumerate(POOL_WINDOWS):
        lo = jnp.clip(t - w // 2, 0, L_)
        hi = jnp.clip(t + w // 2, 0, L_)
        mean = (cs[:, hi, g] - cs[:, lo, g]) / (hi - lo).astype(jnp.float32)[None, :, None]
        diffs.append(mean - hf[:, :, g])
    pooled = jnp.stack(diffs, axis=2).astype(h.dtype)
    y = jnp.einsum('blgc,gce->blge', pooled, w_pool).reshape(B_, L_, D_MODEL)
    return y * scale


def peer_ffn(h, w_q, sub_keys, down, up):
    shape = h.shape
    blocks = h.reshape(-1, PEER_BLOCK, D_MODEL)

    def one_block(xb):
        n = xb.shape[0]
        q = (xb @ w_q).reshape(n, PEER_HEADS, 2, PEER_DHALF)
        s = jnp.einsum('thpk,pnk->thpn', q, sub_keys).astype(jnp.float32)
        s_top, i_top = lax.top_k(s, PEER_TOPK)
        cand_s = s_top[:, :, 0, :, None] + s_top[:, :, 1, None, :]
        cand_i = i_top[:, :, 0, :, None] * PEER_NKEYS + i_top[:, :, 1, None, :]
        best_s, best_pos = lax.top_k(cand_s.reshape(n, PEER_HEADS, -1), PEER_TOPK)
        idx = jnp.take_along_axis(cand_i.reshape(n, PEER_HEADS, -1), best_pos, axis=-1)
        gate = jax.nn.softmax(best_s, axis=-1)
        u = jnp.take(down, idx, axis=0)
        act = jax.nn.gelu(jnp.einsum('thkd,td->thk', u, xb))
        v = jnp.take(up, idx, axis=0)
        return jnp.einsum('thk,thkd->td', (gate * act).astype(v.dtype), v)

    return lax.map(one_block, blocks).reshape(shape)


def setup_inputs(seed: int = 0) -> dict:
    key = jax.random.key(seed)
    ks = jax.random.split(key, 20)
    D = D_MODEL

    def nrm(k, shape, s):
        return jax.random.normal(k, shape, jnp.float32) * s

    return {
        'x': nrm(ks[0], (BATCH, SEQ, D), 1.0),
        'c': nrm(ks[1], (BATCH, D), 1.0),
        'ctx': nrm(ks[2], (BATCH, CTX_LEN, D), 1.0),
        'c_ctx': nrm(ks[3], (D,), 1.0),
        'mod_w': nrm(ks[4], (DEPTH, D, N_MOD * D), 0.5 * D ** -0.5),
        'mod_b': nrm(ks[5], (DEPTH, N_MOD * D), 0.02),
        'norm1_g': 1.0 + nrm(ks[6], (DEPTH, D), 0.02),
        'norm2_g': 1.0 + nrm(ks[7], (DEPTH, D), 0.02),
        'attn_in_w': nrm(ks[8], (N_EVEN, D, IN_W), D ** -0.5),
        'attn_out_w': nrm(ks[9], (N_EVEN, MIX_W, D), MIX_W ** -0.5),
        'attn_sink': nrm(ks[10], (N_EVEN, ATTN_HEADS), 0.5),
        'gmlp_w_s': nrm(ks[11], (N_EVEN, GMLP_GROUPS, CHUNK, CHUNK), CHUNK ** -0.5),
        'gmlp_b_s': 1.0 + nrm(ks[12], (N_EVEN, GMLP_GROUPS, CHUNK), 0.02),
        'pool_w': nrm(ks[13], (N_ODD, POOL_GROUPS, POOL_DIM, POOL_DIM), POOL_DIM ** -0.5),
        'pool_scale': 1.0 + nrm(ks[14], (N_ODD, D), 0.02),
        'peer_w_q': nrm(ks[15], (DEPTH, D, PEER_HEADS * PEER_DKEY), D ** -0.5),
        'peer_sub_keys': nrm(ks[16], (DEPTH, 2, PEER_NKEYS, PEER_DHALF), PEER_DHALF ** -0.5),
        'peer_down': nrm(ks[17], (DEPTH, PEER_EXPERTS, D), D ** -0.5),
        'peer_up': nrm(ks[18], (DEPTH, PEER_EXPERTS, D), 0.5),
        'final_g': 1.0 + nrm(ks[19], (D,), 0.02),
    }


def reference(x, c, ctx, c_ctx, mod_w, mod_b, norm1_g, norm2_g, attn_in_w, attn_out_w, attn_sink,
              gmlp_w_s, gmlp_b_s, pool_w, pool_scale, peer_w_q, peer_sub_keys, peer_down, peer_up, final_g):
    L_ = x.shape[1]
    ROWS = L_ // GRID_W
    t = jnp.arange(ROWS * GRID_W)
    row = (t // GRID_W).astype(jnp.float32)
    col = (t % GRID_W).astype(jnp.float32)
    last_reader = ((DEPTH - 1) // 2) * 2
    xc = ctx
    for l in range(DEPTH):
        has_ctx = l <= last_reader
        keep_ctx = l < last_reader
        m = jax.nn.silu(c) @ mod_w[l] + mod_b[l]
        sh1, sc1, g1, sh2, sc2, g2 = jnp.split(m[:, None, :], N_MOD, axis=-1)
        h = rms_norm(x, norm1_g[l]) * (1.0 + sc1) + sh1
        hc = None
        if has_ctx:
            mc = jax.nn.silu(c_ctx) @ mod_w[l] + mod_b[l]
            csh1, csc1, cg1, csh2, csc2, cg2 = jnp.split(mc, N_MOD, axis=-1)
            hc = rms_norm(xc, norm1_g[l]) * (1.0 + csc1) + csh1
        if l % 2 == 0:
            e = l // 2
            y, yc = attn_gmlp_mixer(h, hc, attn_in_w[e], attn_out_w[e], attn_sink[e],
                                    gmlp_w_s[e], gmlp_b_s[e], row, col, keep_ctx)
        else:
            o = l // 2
            y = multiscale_pool(h, pool_w[o], pool_scale[o])
            yc = multiscale_pool(hc, pool_w[o], pool_scale[o]) if keep_ctx else None
        x = x + g1 * y
        h2 = rms_norm(x, norm2_g[l]) * (1.0 + sc2) + sh2
        x = x + g2 * peer_ffn(h2, peer_w_q[l], peer_sub_keys[l], peer_down[l], peer_up[l])
        if keep_ctx:
            xc = xc + cg1 * yc
            hc2 = rms_norm(xc, norm2_g[l]) * (1.0 + csc2) + csh2
            xc = xc + cg2 * peer_ffn(hc2, peer_w_q[l], peer_sub_keys[l], peer_down[l], peer_up[l])
    return rms_norm(x, final_g)
```

```python
import numpy as np
from contextlib import ExitStack
import concourse.bass as bass
import concourse.mybir as mybir
from concourse.bass_utils import run_bass_kernel_spmd

F32 = mybir.dt.float32
U32 = mybir.dt.uint32
BF16 = mybir.dt.bfloat16
AF = mybir.ActivationFunctionType
ALU = mybir.AluOpType
AX = mybir.AxisListType

D = 1024
NCORE = 8
OWN = 4096
NOWN = 32
NEXT = 36
EPS = 1e-6
GELU = AF.Gelu_apprx_tanh
NEG = -1e30


class Sem:
    __slots__ = ("h", "val", "name")

    def __init__(self, h, name):
        self.h = h
        self.val = 0
        self.name = name


class Buf:
    __slots__ = ("name", "w", "r")

    def __init__(self, name):
        self.name = name
        self.w = None
        self.r = {}


def _b(x):
    return x if isinstance(x, Buf) else x.b


ENGS = ("pe", "act", "dve", "pool", "sp")


class Prog:
    def __init__(self, nc, stack, n_dsem=64):
        self.nc = nc
        self.q = {e: [] for e in ENGS}
        self.esem = {}
        for e in ENGS:
            if e != "sp":
                self.esem[e] = Sem(stack.enter_context(nc.semaphore("e_" + e)), e)
        self.dpool = [Sem(stack.enter_context(nc.semaphore("d%d" % i)), "d%d" % i) for i in range(n_dsem)]
        self.dfree = list(self.dpool)
        self.seen = {e: {} for e in ENGS}
        self.nblock = 0

    def get_dsem(self):
        return self.dfree.pop()

    def op(self, eng, fn, reads=(), writes=(), dsem=None):
        need = {}
        for x in reads:
            b = _b(x)
            if b.w is not None:
                s, v = b.w
                if need.get(s, 0) < v:
                    need[s] = v
        for x in writes:
            b = _b(x)
            if b.w is not None:
                s, v = b.w
                if need.get(s, 0) < v:
                    need[s] = v
            for s, v in b.r.items():
                if need.get(s, 0) < v:
                    need[s] = v
        own = self.esem.get(eng)
        seen = self.seen[eng]
        q = self.q[eng]
        for s, v in need.items():
            if eng == "pe" and s is own:
                continue
            if seen.get(s, 0) >= v:
                continue
            seen[s] = v
            q.append(("w", s, v))
        if dsem is not None:
            sem = dsem
            sem.val += 16
            inc = 16
        else:
            sem = own
            sem.val += 1
            inc = 1
        q.append(("o", fn, sem, inc))
        for x in reads:
            b = _b(x)
            if b.r.get(sem, 0) < sem.val:
                b.r[sem] = sem.val
        tag = (sem, sem.val)
        for x in writes:
            b = _b(x)
            b.w = tag
            b.r = {}

    def end_phase(self):
        allsems = list(self.esem.values()) + self.dpool
        for e in ENGS:
            seen = self.seen[e]
            for s in allsems:
                if s.val > 0 and seen.get(s, 0) < s.val:
                    seen[s] = s.val
                    self.q[e].append(("w", s, s.val))
        nc = self.nc
        q = self.q
        with nc.Block() as block:
            def run(engine, items):
                for it in items:
                    if it[0] == "w":
                        engine.wait_ge(it[1].h, it[2])
                    else:
                        it[1](engine).then_inc(it[2].h, it[3])

            @block.tensor
            def _(e):
                run(e, q["pe"])

            @block.scalar
            def _(e):
                run(e, q["act"])

            @block.vector
            def _(e):
                run(e, q["dve"])

            @block.gpsimd
            def _(e):
                run(e, q["pool"])

            @block.sync
            def _(e):
                run(e, q["sp"])
        self.q = {e: [] for e in ENGS}
        self.dfree = list(self.dpool)
        self.nblock += 1


class T:
    def __init__(self, K, st, name, shape, dt=F32, psum=False):
        K.uid += 1
        nm = "%s_%d" % (name, K.uid)
        alloc = K.nc.psum_tensor if psum else K.nc.sbuf_tensor
        self.t = st.enter_context(alloc(nm, shape, dt))
        self.b = Buf(nm)
        self.K = K
        self._sem = None

    def __getitem__(self, k):
        return self.t[k]

    @property
    def sem(self):
        if self._sem is None:
            self._sem = self.K.P.get_dsem()
        return self._sem


class Dr:
    def __init__(self, ap, name):
        self.ap = ap
        self.b = Buf(name)


class Kern:
    def __init__(self, nc, gst):
        self.nc = nc
        self.uid = 0
        self.P = Prog(nc, gst)

    def dma(self, out, in_, reads, writes, sem, eng="sp"):
        self.P.op(eng, lambda e: e.dma_start(out=out, in_=in_), reads, writes, dsem=sem)

    def mm(self, out, lhsT, rhs, start, stop, reads, writes, skip=False):
        self.P.op("pe", lambda e: e.matmul(out, lhsT=lhsT, rhs=rhs, start=start, stop=stop, skip_group_check=skip), reads, writes)

    def tr(self, out, in_, ident, reads, writes):
        self.P.op("pe", lambda e: e.transpose(out=out, in_=in_, identity=ident), reads, writes)

    def act(self, out, in_, func, reads, writes, **kw):
        self.P.op("act", lambda e: e.activation(out=out, in_=in_, func=func, **kw), reads, writes)

    def acopy(self, out, in_, reads, writes):
        self.P.op("act", lambda e: e.copy(out=out, in_=in_), reads, writes)

    def tt(self, out, in0, in1, op, reads, writes, eng="dve"):
        self.P.op(eng, lambda e: e.tensor_tensor(out=out, in0=in0, in1=in1, op=op), reads, writes)

    def ts(self, out, in0, s1, s2, op0, op1, reads, writes, eng="dve"):
        if op1 is None:
            self.P.op(eng, lambda e: e.tensor_scalar(out=out, in0=in0, scalar1=s1, scalar2=None, op0=op0), reads, writes)
        else:
            self.P.op(eng, lambda e: e.tensor_scalar(out=out, in0=in0, scalar1=s1, scalar2=s2, op0=op0, op1=op1), reads, writes)

    def stt(self, out, in0, scalar, in1, op0, op1, reads, writes):
        self.P.op("dve", lambda e: e.scalar_tensor_tensor(out=out, in0=in0, scalar=scalar, in1=in1, op0=op0, op1=op1), reads, writes)

    def cp(self, out, in_, reads, writes, eng="dve"):
        self.P.op(eng, lambda e: e.tensor_copy(out=out, in_=in_), reads, writes)

    def recip(self, out, in_, reads, writes):
        self.P.op("dve", lambda e: e.reciprocal(out=out, in_=in_), reads, writes)

    def memset(self, out, val, writes, eng="pool"):
        self.P.op(eng, lambda e: e.memset(out, val), (), writes)

    def red(self, out, in_, op, reads, writes):
        self.P.op("dve", lambda e: e.tensor_reduce(out=out, in_=in_, axis=AX.X, op=op), reads, writes)

    def ttr(self, out, in0, in1, accum, reads, writes):
        self.P.op("dve", lambda e: e.scalar_tensor_tensor(out=out, in0=in0, scalar=1.0, in1=in1, op0=ALU.mult, op1=ALU.mult,
                                                         accum_out=accum), reads, writes)

    def vmax(self, out, in_, reads, writes):
        self.P.op("dve", lambda e: e.max(out=out, in_=in_), reads, writes)

    def vmaxidx(self, out, in_max, in_values, reads, writes):
        self.P.op("dve", lambda e: e.max_index(out=out, in_max=in_max, in_values=in_values), reads, writes)

    def vmr(self, out, rep, vals, reads, writes):
        self.P.op("dve", lambda e: e.match_replace(out=out, in_to_replace=rep, in_values=vals, imm_value=NEG), reads, writes)

    def gather(self, out, tab, idx, reads, writes, sem):
        self.P.op("pool", lambda e: e.indirect_dma_start(out=out, out_offset=None, in_=tab,
                                                         in_offset=bass.IndirectOffsetOnAxis(ap=idx, axis=0)),
                  reads, writes, dsem=sem)

    def rstd_of(self, xt, junk, ss, rstd):
        self.act(junk[:, 0:D], xt[:, :], AF.Square, [xt], [junk, ss], accum_out=ss[:, 0:1])
        self.act(rstd[:, 0:1], ss[:, 0:1], AF.Sqrt, [ss], [rstd], scale=1.0 / D, bias=EPS)
        self.recip(rstd[:, 0:1], rstd[:, 0:1], [rstd], [rstd])

    def norm_mod(self, xt, h, SC, SH, junk, ss, rstd):
        self.rstd_of(xt, junk, ss, rstd)
        self.stt(h[:, :], xt[:, :], rstd[:, 0:1], SC[:, :], ALU.mult, ALU.mult, [xt, rstd, SC], [h])
        self.tt(h[:, :], h[:, :], SH[:, :], ALU.add, [h, SH], [h])

    def transpose8(self, src, dst, ident, banks):
        for half in range(2):
            bk = banks[half]
            for k in range(4):
                kk = half * 4 + k
                self.tr(bk[:, k * 128:(k + 1) * 128], src[:, kk * 128:(kk + 1) * 128], ident[:, :], [src, ident], [bk])
            self.acopy(dst[:, half * 512:(half + 1) * 512], bk[:, :], [bk], [dst])


def build_nc(debug=False):
    nc = bass.Bass("TRN2", target_bir_lowering=False)

    def din(name, shape, dt=F32):
        return nc.dram_tensor(name, shape, dt, kind="ExternalInput").ap()

    skind = "ExternalOutput" if debug else "Internal"

    def dscr(name, shape):
        return nc.dram_tensor(name, shape, F32, kind=skind).ap()

    x_ext = din("x_ext", [NEXT * 128, D])
    ctx_b = din("ctx_b", [256, D])
    c_cols = din("c_cols", [128, 16])
    rope = din("rope", [NEXT * 128, 128])
    kvalid_d = din("kvalid", [128, NEXT])
    poolBT = din("poolBT", [128, 36, 128])
    ident_d = din("ident", [128, 128])
    masks_d = din("masks", [128, 2, 128])
    iota_d = din("iota16", [128, 16])
    mod_w = din("mod_w", [2, D, 6 * D])
    mod_b = din("mod_b", [2, 6 * D])
    norm1_g = din("norm1_g", [2, D])
    norm2_g = din("norm2_g", [2, D])
    w_in = din("attn_in_w", [D, 1792])
    w_out = din("attn_out_w", [D, D])
    sink_d = din("attn_sink", [8])
    w_s = din("gmlp_w_s", [8, 128, 128])
    bs_col = din("bs_col", [128, 8])
    pool_w = din("pool_w", [4, 256, 256])
    pool_scale = din("pool_scale", [D])
    w_q = din("peer_w_q", [2, D, 2048])
    subk = din("peer_sub_keys", [2, 2, 128, 128])
    tab = [din("peer_tab0", [16384, 2048]), din("peer_tab1", [16384, 2048])]
    final_g = din("final_g", [D])
    out_d = nc.dram_tensor("out", [OWN, D], F32, kind="ExternalOutput").ap()

    mods_d = dscr("mods", [2, 8, 128, D])
    xmid0_d = dscr("xmid0", [NEXT * 128, D])
    x1_d = dscr("x1", [NEXT * 128, D])
    xmid1_d = dscr("xmid1", [OWN, D])
    tabbf = [nc.dram_tensor("tabbf%d" % l, [16384, 2048], BF16, kind=skind).ap() for l in range(2)]

    with ExitStack() as G:
        K = Kern(nc, G)
        P = K.P
        bank = [T(K, G, "bank%d" % i, [128, 512], F32, psum=True) for i in range(8)]
        ident = T(K, G, "ident", [128, 128])
        masks = T(K, G, "masks", [128, 256])
        iota16 = T(K, G, "iota16", [128, 16])
        kvalid = T(K, G, "kvalid", [128, NEXT])
        KcT = T(K, G, "KcT", [64, 512], BF16)
        Vc = [T(K, G, "Vc%d" % i, [128, 130], BF16) for i in range(2)]

        MODS = [[Dr(mods_d[l, i], "mods%d_%d" % (l, i)) for i in range(8)] for l in range(2)]
        XMID0 = [Dr(xmid0_d[e * 128:(e + 1) * 128, :], "xmid0_%d" % e) for e in range(NEXT)]
        X1 = [Dr(x1_d[e * 128:(e + 1) * 128, :], "x1_%d" % e) for e in range(NEXT)]
        XMID1 = [Dr(xmid1_d[j * 128:(j + 1) * 128, :], "xmid1_%d" % j) for j in range(NOWN)]
        OUT = [Dr(out_d[j * 128:(j + 1) * 128, :], "out_%d" % j) for j in range(NOWN)]
        TABBF = [Buf("tabbf0"), Buf("tabbf1")]

        K.dma(ident[:, :], ident_d, [], [ident], ident.sem)
        K.dma(masks[:, :], masks_d.rearrange("p a b -> p (a b)"), [], [masks], masks.sem)
        K.dma(iota16[:, :], iota_d, [], [iota16], iota16.sem)
        K.dma(kvalid[:, :], kvalid_d, [], [kvalid], kvalid.sem)

        def phase_mods(l):
            with ExitStack() as st:
                cc = T(K, st, "cc", [128, 16])
                sil = T(K, st, "sil", [128, 16])
                cb = T(K, st, "cb", [128, 16 * 128])
                modb = T(K, st, "modb", [128, 6 * D])
                gb1 = T(K, st, "gb1", [128, D])
                gb2 = T(K, st, "gb2", [128, D])
                psb = T(K, st, "psb", [128, D])
                wsl = [T(K, st, "wsl%d" % i, [128, 2048]) for i in range(3)]
                res = [T(K, st, "res%d" % i, [128, 512]) for i in range(4)]
                K.dma(cc[:, :], c_cols, [], [cc], cc.sem)
                K.dma(modb[:, :], mod_b[l, :].partition_broadcast(128), [], [modb], modb.sem)
                K.dma(gb1[:, :], norm1_g[l, :].partition_broadcast(128), [], [gb1], gb1.sem)
                K.dma(gb2[:, :], norm2_g[l, :].partition_broadcast(128), [], [gb2], gb2.sem)
                if l == 1:
                    K.dma(psb[:, :], pool_scale.partition_broadcast(128), [], [psb], psb.sem)
                K.act(sil[:, :], cc[:, :], AF.Silu, [cc], [sil])
                K.cp(cb[:, :].rearrange("p (k m) -> p k m", m=128), sil[:, :].unsqueeze(2).to_broadcast([128, 16, 128]), [sil], [cb])
                nload = 0
                nres = 0
                for p in range(3):
                    do_ctx = (l == 0 and p == 0)
                    for k in range(8):
                        ws = wsl[nload % 3]
                        nload += 1
                        K.dma(ws[:, :], mod_w[l, k * 128:(k + 1) * 128, p * 2048:(p + 1) * 2048], [], [ws], ws.sem)
                        for j in range(4):
                            K.mm(bank[j][:, :], cb[:, k * 128:(k + 1) * 128], ws[:, j * 512:(j + 1) * 512], k == 0, k == 7, [cb, ws], [bank[j]])
                        if do_ctx:
                            for j in range(4):
                                K.mm(bank[4 + j][:, :], cb[:, (8 + k) * 128:(9 + k) * 128], ws[:, j * 512:(j + 1) * 512], k == 0, k == 7, [cb, ws], [bank[4 + j]])
                    for isctx in ([False, True] if do_ctx else [False]):
                        for j in range(4):
                            cbk = p * 4 + j
                            mi = cbk // 2
                            half = cbk % 2
                            hs = slice(half * 512, (half + 1) * 512)
                            bk = bank[4 + j] if isctx else bank[j]
                            r = res[nres % 4]
                            nres += 1
                            K.tt(r[:, :], bk[:, :], modb[:, cbk * 512:(cbk + 1) * 512], ALU.add, [bk, modb], [r])
                            if mi in (1, 4):
                                gb = gb1 if mi == 1 else gb2
                                K.stt(r[:, :], r[:, :], 1.0, gb[:, hs], ALU.add, ALU.mult, [r, gb], [r])
                            if l == 1 and mi == 2:
                                K.tt(r[:, :], r[:, :], psb[:, hs], ALU.mult, [r, psb], [r])
                            dst = MODS[l][6 + mi] if isctx else MODS[l][mi]
                            K.dma(dst.ap[:, hs], r[:, :], [r], [dst], r.sem)
                P.end_phase()

        def phase_ctx():
            with ExitStack() as st:
                CSH = T(K, st, "CSH", [128, D])
                CSC = T(K, st, "CSC", [128, D])
                wkv = T(K, st, "wkv", [128, 8 * 256], BF16)
                xt = T(K, st, "xt", [128, D])
                h = T(K, st, "h", [128, D])
                hT = T(K, st, "hT", [128, D], BF16)
                junk = h
                ss = T(K, st, "ss", [128, 1])
                rstd = T(K, st, "rstd", [128, 1])
                ktmp = T(K, st, "ktmp", [128, 128])
                K.dma(CSH[:, :], MODS[0][6].ap, [MODS[0][6]], [CSH], CSH.sem)
                K.dma(CSC[:, :], MODS[0][7].ap, [MODS[0][7]], [CSC], CSC.sem)
                K.dma(wkv[:, :].rearrange("p (k n) -> p k n", n=256), w_in.rearrange("(k p) n -> p k n", p=128)[:, :, 512:768], [], [wkv], wkv.sem, eng="pool")
                for ct in range(2):
                    K.memset(Vc[ct][:, :], 1.0, [Vc[ct]])
                for ct in range(2):
                    K.dma(xt[:, :], ctx_b[ct * 128:(ct + 1) * 128, :], [], [xt], xt.sem)
                    K.norm_mod(xt, h, CSC, CSH, junk, ss, rstd)
                    K.transpose8(h, hT, ident, [bank[0], bank[1]])
                    for k in range(8):
                        K.mm(bank[2][:, 0:256], hT[:, k * 128:(k + 1) * 128], wkv[:, k * 256:(k + 1) * 256], k == 0, k == 7, [hT, wkv], [bank[2]])
                    K.cp(Vc[ct][:, :].rearrange("p (g d) -> p g d", d=65)[:, :, 0:64],
                         bank[2][:, 128:256].rearrange("p (g d) -> p g d", d=64), [bank[2]], [Vc[ct]])
                    K.acopy(ktmp[:, :], bank[2][:, 0:128], [bank[2]], [ktmp])
                    for g in range(2):
                        K.tr(bank[3][0:64, g * 128:(g + 1) * 128], ktmp[:, g * 64:(g + 1) * 64], ident[:, :], [ktmp, ident], [bank[3]])
                    K.cp(KcT[:, :].rearrange("p (g c j) -> p g c j", g=2, c=2)[:, :, ct, :],
                         bank[3][0:64, 0:256].rearrange("p (g j) -> p g j", g=2), [bank[3]], [KcT])
                P.end_phase()

        def phase_mixer0():
            with ExitStack() as st:
                SH1 = T(K, st, "SH1", [128, D]); SC1 = T(K, st, "SC1", [128, D]); G1 = T(K, st, "G1", [128, D])
                win = T(K, st, "win", [128, 8 * 1792], BF16)
                wout = T(K, st, "wout", [128, 8 * 1024], BF16)
                wsT = T(K, st, "wsT", [128, 8 * 128], BF16)
                bsc = T(K, st, "bsc", [128, 8])
                esink = T(K, st, "esink", [128, 8])
                xts = [T(K, st, "xt%d" % i, [128, D]) for i in range(3)]
                rps = [T(K, st, "rp%d" % i, [128, 128]) for i in range(2)]
                h = T(K, st, "h", [128, D])
                hT = T(K, st, "hT", [128, D], BF16)
                junk = h
                ss = T(K, st, "ss", [128, 1]); rstd = T(K, st, "rstd", [128, 1])
                qr = T(K, st, "qr", [128, 512]); qs = T(K, st, "qs", [128, 512])
                kr = T(K, st, "kr", [128, 128]); ks = T(K, st, "ks", [128, 128])
                QT = [T(K, st, "QT%d" % i, [64, 1024], BF16) for i in range(3)]
                KT = [T(K, st, "KT%d" % i, [64, 256], BF16) for i in range(4)]
                Vr = [T(K, st, "Vr%d" % i, [128, 130], BF16) for i in range(4)]
                U = [T(K, st, "U%d" % i, [128, 512]) for i in range(3)]
                VN = [T(K, st, "VN%d" % i, [128, 512], BF16) for i in range(3)]
                vg = T(K, st, "vg", [128, 512]); cen = T(K, st, "cen", [128, 512]); sq = vg
                mu = T(K, st, "mu", [128, 8]); var = T(K, st, "var", [128, 8])
                pts = [T(K, st, "pt%d" % i, [128, 512], BF16) for i in range(4)]
                den = T(K, st, "den", [128, 8]); rden = T(K, st, "rden", [128, 8])
                cat = T(K, st, "cat", [128, D]); catT = T(K, st, "catT", [128, D], BF16)
                tmp = T(K, st, "tmp", [128, 512])
                wsl = cat

                for (t_, m_) in ((SH1, 0), (SC1, 1), (G1, 2)):
                    K.dma(t_[:, :], MODS[0][m_].ap, [MODS[0][m_]], [t_], t_.sem)
                K.dma(win[:, :].rearrange("p (k n) -> p k n", n=1792), w_in.rearrange("(k p) n -> p k n", p=128), [], [win], win.sem, eng="pool")
                K.dma(wout[:, :].rearrange("p (k n) -> p k n", n=1024), w_out.rearrange("(k p) n -> p k n", p=128), [], [wout], wout.sem, eng="pool")
                K.dma(wsl[:, :].rearrange("p (g q) -> p g q", q=128), w_s.rearrange("g p q -> p g q"), [], [wsl], wsl.sem)
                K.dma(bsc[:, :], bs_col, [], [bsc], bsc.sem)
                K.dma(esink[:, :], sink_d.partition_broadcast(128), [], [esink], esink.sem)
                K.act(esink[:, :], esink[:, :], AF.Exp, [esink], [esink])
                for g in range(8):
                    bk = bank[g // 4]
                    K.tr(bk[:, (g % 4) * 128:(g % 4 + 1) * 128], wsl[:, g * 128:(g + 1) * 128], ident[:, :], [wsl, ident], [bk])
                    if g % 4 == 3:
                        K.acopy(wsT[:, (g // 4) * 512:(g // 4 + 1) * 512], bk[:, :], [bk], [wsT])

                def do_rope(src_ap, nh, cr, cs, rp, srcb):
                    v5 = "p (h f two d) -> p h f two d"
                    K.tt(cr[:, :].rearrange("p (h d) -> p h d", d=64), src_ap.rearrange("p (h d) -> p h d", d=64),
                         rp[:, 0:64].unsqueeze(1).to_broadcast([128, nh, 64]), ALU.mult, [srcb, rp], [cr])
                    for j in range(2):
                        K.tt(cs[:, :].rearrange(v5, h=nh, f=2, two=2)[:, :, :, j, :],
                             src_ap.rearrange(v5, h=nh, f=2, two=2)[:, :, :, 1 - j, :],
                             rp[:, 64:128].rearrange("p (f two d) -> p f two d", f=2, two=2)[:, :, j, :].unsqueeze(1).to_broadcast([128, nh, 2, 16]),
                             ALU.mult, [srcb, rp], [cs])
                    K.tt(cr[:, :], cr[:, :], cs[:, :], ALU.add, [cr, cs], [cr])

                def stageA(e):
                    full = 1 <= e <= NEXT - 2
                    xt = xts[e % 3]
                    rp = rps[e % 2]
                    K.dma(xt[:, :], x_ext[e * 128:(e + 1) * 128, :], [], [xt], xt.sem)
                    K.dma(rp[:, :], rope[e * 128:(e + 1) * 128, :], [], [rp], rp.sem)
                    yield
                    K.norm_mod(xt, h, SC1, SH1, junk, ss, rstd)
                    yield
                    K.transpose8(h, hT, ident, [bank[0], bank[1]])
                    yield
                    for k in range(8):
                        lt = hT[:, k * 128:(k + 1) * 128]
                        wb = k * 1792
                        if full:
                            K.mm(bank[2][:, :], lt, win[:, wb:wb + 512], k == 0, k == 7, [hT, win], [bank[2]])
                        K.mm(bank[3][:, 0:256], lt, win[:, wb + 512:wb + 768], k == 0, k == 7, [hT, win], [bank[3]])
                        if full:
                            K.mm(bank[0][:, :], lt, win[:, wb + 768:wb + 1280], k == 0, k == 7, [hT, win], [bank[0]])
                            K.mm(bank[1][:, :], lt, win[:, wb + 1280:wb + 1792], k == 0, k == 7, [hT, win], [bank[1]])
                        if k % 2 == 1:
                            yield
                    do_rope(bank[3][:, 0:128], 2, kr, ks, rp, bank[3])
                    vr = Vr[e % 4]
                    K.ts(vr[:, :].rearrange("p (g d) -> p g d", d=65)[:, :, 0:64], bank[3][:, 128:256].rearrange("p (g d) -> p g d", d=64),
                         kvalid[:, e:e + 1], None, ALU.mult, None, [bank[3], kvalid], [vr])
                    K.cp(vr[:, :].rearrange("p (g d) -> p g d", d=65)[:, :, 64:65], kvalid[:, e:e + 1].unsqueeze(1).to_broadcast([128, 2, 1]), [kvalid, vr], [vr])
                    yield
                    kt = KT[e % 4]
                    for g in range(2):
                        K.tr(bank[3][0:64, g * 128:(g + 1) * 128], kr[:, g * 64:(g + 1) * 64], ident[:, :], [kr, ident], [bank[3]])
                    K.acopy(kt[:, :], bank[3][0:64, 0:256], [bank[3]], [kt])
                    yield
                    if not full:
                        return
                    u = U[e % 3]
                    vn = VN[e % 3]
                    K.act(u[:, :], bank[0][:, :], GELU, [bank[0]], [u])
                    K.act(vg[:, :], bank[1][:, :], GELU, [bank[1]], [vg])
                    yield
                    do_rope(bank[2][:, :], 8, qr, qs, rp, bank[2])
                    yield
                    qt = QT[e % 3]
                    for hh in range(8):
                        bk = bank[2 * (hh // 4)]
                        K.tr(bk[0:64, (hh % 4) * 128:(hh % 4 + 1) * 128], qr[:, hh * 64:(hh + 1) * 64], ident[:, :], [qr, ident], [bk])
                        if hh % 4 == 3:
                            K.acopy(qt[:, (hh // 4) * 512:(hh // 4 + 1) * 512], bk[0:64, :], [bk], [qt])
                            yield
                    g3 = "p (g d) -> p g d"
                    K.red(mu[:, :], vg[:, :].rearrange(g3, d=64), ALU.add, [vg], [mu])
                    K.ts(mu[:, :], mu[:, :], 1.0 / 64, None, ALU.mult, None, [mu], [mu])
                    K.tt(cen[:, :].rearrange(g3, d=64), vg[:, :].rearrange(g3, d=64), mu[:, :].unsqueeze(2).to_broadcast([128, 8, 64]), ALU.subtract, [vg, mu], [cen])
                    yield
                    K.tt(sq[:, :], cen[:, :], cen[:, :], ALU.mult, [cen], [sq])
                    K.red(var[:, :], sq[:, :].rearrange(g3, d=64), ALU.add, [sq], [var])
                    K.act(var[:, :], var[:, :], AF.Sqrt, [var], [var], scale=1.0 / 64, bias=EPS)
                    K.recip(var[:, :], var[:, :], [var], [var])
                    yield
                    K.tt(vn[:, :].rearrange(g3, d=64), cen[:, :].rearrange(g3, d=64), var[:, :].unsqueeze(2).to_broadcast([128, 8, 64]), ALU.mult, [cen, var], [vn])
                    yield

                npt = [0]

                def stageB(e, tick):
                    xt = xts[e % 3]
                    qt = QT[e % 3]
                    steps = []
                    for gk in range(2):
                        for ti, (kind, s_, mk) in enumerate([("w", e - 1, 0), ("w", e, None), ("w", e + 1, 1), ("c", 0, None), ("c", 1, None)]):
                            steps.append((gk, ti, kind, s_, mk))

                    def srcs(gk, kind, s_):
                        if kind == "w":
                            ksrc = KT[s_ % 4]
                            return ksrc, ksrc[:, gk * 128:(gk + 1) * 128], Vr[s_ % 4]
                        return KcT, KcT[:, gk * 256 + s_ * 128:gk * 256 + (s_ + 1) * 128], Vc[s_]

                    def score(i):
                        gk, ti, kind, s_, mk = steps[i]
                        ksrc, lhs, vsrc = srcs(gk, kind, s_)
                        sb_ = bank[4 + (i % 2)]
                        K.mm(sb_[:, :], lhs, qt[:, gk * 512:(gk + 1) * 512], True, True, [ksrc, qt], [sb_])

                    score(0)
                    for i, (gk, ti, kind, s_, mk) in enumerate(steps):
                        ksrc, lhs, vsrc = srcs(gk, kind, s_)
                        ob = bank[6 + gk]
                        sb_ = bank[4 + (i % 2)]
                        pt = pts[npt[0] % 4]
                        npt[0] += 1
                        if i + 1 < len(steps):
                            score(i + 1)
                        K.act(pt[:, :], sb_[:, :], AF.Exp, [sb_], [pt], scale=0.125)
                        if mk is not None:
                            K.tt(pt[:, :].rearrange("p (h q) -> p h q", q=128), pt[:, :].rearrange("p (h q) -> p h q", q=128),
                                 masks[:, mk * 128:(mk + 1) * 128].unsqueeze(1).to_broadcast([128, 4, 128]), ALU.mult, [pt, masks], [pt])
                        for hh in range(4):
                            K.mm(ob[:, hh * 65:(hh + 1) * 65], pt[:, hh * 128:(hh + 1) * 128], vsrc[:, gk * 65:(gk + 1) * 65],
                                 ti == 0 and hh == 0, ti == 4, [pt, vsrc], [ob], skip=True)
                        tick()
                        if ti == 4:
                            o3 = ob[:, 0:260].rearrange("p (h d) -> p h d", d=65)
                            K.tt(den[:, gk * 4:(gk + 1) * 4].unsqueeze(2), o3[:, :, 64:65], esink[:, gk * 4:(gk + 1) * 4].unsqueeze(2), ALU.add, [ob, esink], [den])
                            K.recip(rden[:, gk * 4:(gk + 1) * 4], den[:, gk * 4:(gk + 1) * 4], [den], [rden])
                            K.tt(cat[:, gk * 256:(gk + 1) * 256].rearrange("p (h d) -> p h d", d=64), o3[:, :, 0:64],
                                 rden[:, gk * 4:(gk + 1) * 4].unsqueeze(2).to_broadcast([128, 4, 64]), ALU.mult, [ob, rden], [cat])
                    vn = VN[e % 3]
                    u = U[e % 3]
                    for g in range(8):
                        K.mm(bank[4][:, g * 64:(g + 1) * 64], wsT[:, g * 128:(g + 1) * 128], vn[:, g * 64:(g + 1) * 64], True, True, [wsT, vn], [bank[4]])
                    K.tt(tmp[:, :].rearrange("p (g d) -> p g d", d=64), bank[4][:, :].rearrange("p (g d) -> p g d", d=64),
                         bsc[:, :].unsqueeze(2).to_broadcast([128, 8, 64]), ALU.add, [bank[4], bsc], [tmp])
                    K.tt(cat[:, 512:1024], tmp[:, :], u[:, :], ALU.mult, [tmp, u], [cat])
                    tick()
                    K.transpose8(cat, catT, ident, [bank[4], bank[5]])
                    tick()
                    for n in range(2):
                        yb = bank[6 + n]
                        for k in range(8):
                            K.mm(yb[:, :], catT[:, k * 128:(k + 1) * 128], wout[:, k * 1024 + n * 512:k * 1024 + (n + 1) * 512], k == 0, k == 7, [catT, wout], [yb])
                        cs = slice(n * 512, (n + 1) * 512)
                        K.tt(tmp[:, :], yb[:, :], G1[:, cs], ALU.mult, [yb, G1], [tmp])
                        K.tt(xt[:, cs], tmp[:, :], xt[:, cs], ALU.add, [tmp, xt], [xt])
                        tick()
                    K.dma(XMID0[e].ap, xt[:, :], [xt], [XMID0[e]], xt.sem)

                def conv2():
                    for _ in range(2):
                        if conv_jobs:
                            conv_jobs.pop(0)()

                for e in range(3):
                    conv2()
                    for _ in stageA(e):
                        pass
                for e in range(1, NEXT - 1):
                    conv2()
                    gen = stageA(e + 2) if e + 2 < NEXT else iter(())

                    def tick(gen=gen):
                        next(gen, None)
                        next(gen, None)

                    stageB(e, tick)
                    for _ in gen:
                        pass
                while conv_jobs:
                    conv_jobs.pop(0)()
                P.end_phase()

        def phase_tabconv(l):
            with ExitStack() as st:
                ins = [T(K, st, "cin%d" % i, [128, 8192]) for i in range(2)]
                outs = [T(K, st, "cout%d" % i, [128, 8192], BF16) for i in range(2)]
                for i in range(32):
                    a = ins[i % 2]
                    o = outs[i % 2]
                    K.dma(a[:, :], tab[l][i * 512:(i + 1) * 512, :].rearrange("(p r) c -> p (r c)", r=4), [], [a], a.sem)
                    K.acopy(o[:, 0:3072], a[:, 0:3072], [a], [o])
                    K.cp(o[:, 3072:6144], a[:, 3072:6144], [a], [o])
                    K.cp(o[:, 6144:8192], a[:, 6144:8192], [a], [o], eng="pool")
                    K.dma(tabbf[l][i * 512:(i + 1) * 512, :].rearrange("(p r) c -> p (r c)", r=4), o[:, :], [o], [TABBF[l]], o.sem)
                P.end_phase()

        def phase_peer(l, tiles, src, dst, final):
            with ExitStack() as st:
                SH2 = T(K, st, "SH2", [128, D]); SC2 = T(K, st, "SC2", [128, D]); G2 = T(K, st, "G2", [128, D])
                wq = T(K, st, "wq", [128, 8 * 2048])
                skl = T(K, st, "skl", [128, 256])
                skT = T(K, st, "skT", [128, 256])
                fg = T(K, st, "fg", [128, D]) if final else None
                xts = [T(K, st, "xt%d" % i, [128, D]) for i in range(3)]
                h2s = [T(K, st, "h2_0", [128, D])] * 2
                h2bs = [T(K, st, "h2b_%d" % i, [128, D], BF16) for i in range(2)]
                tmpb = T(K, st, "tmpb", [128, D], BF16)
                ss = T(K, st, "ss", [128, 1]); rstd = T(K, st, "rstd", [128, 1])
                ss2 = T(K, st, "ss2", [128, 1]); rstd2 = T(K, st, "rstd2", [128, 1])
                S0 = T(K, st, "S0", [128, 2048]); S1 = T(K, st, "S1", [128, 2048])
                S2 = T(K, st, "S2", [128, 2048]); S3 = T(K, st, "S3", [128, 2048])
                top = T(K, st, "top", [128, 256]); tidx = T(K, st, "tidx", [128, 256], U32)
                tidxf = T(K, st, "tidxf", [128, 256])
                best = T(K, st, "best", [128, 128]); pos = T(K, st, "pos", [128, 128], U32)
                pa = T(K, st, "pa", [128, 128], U32); pb = T(K, st, "pb", [128, 128], U32)
                sel1 = T(K, st, "sel1", [128, 128]); sel2 = T(K, st, "sel2", [128, 128])
                eidxs = [T(K, st, "eidx%d" % i, [128, 128], U32) for i in range(2)]
                gates = [T(K, st, "gate%d" % i, [128, 128]) for i in range(2)]
                gsum = T(K, st, "gsum", [128, 8])
                dd = T(K, st, "dd", [128, 128]); gd = T(K, st, "gd", [128, 128]); coef = T(K, st, "coef", [128, 128])
                NR = 13 if final else 14
                rows = [T(K, st, "rows%d" % i, [128, 2048], BF16) for i in range(NR)]
                tmp = T(K, st, "tmp", [128, D])
                identb = T(K, st, "identb", [128, 128], BF16)
                diags = [T(K, st, "diag%d" % i, [128, 128], BF16) for i in range(4)]
                K.cp(identb[:, :], ident[:, :], [ident], [identb])
                ddb = [Buf("dd%d" % j) for j in range(128)]
                gdb = [Buf("gd%d" % j) for j in range(128)]

                for (t_, m_) in ((SH2, 3), (SC2, 4), (G2, 5)):
                    K.dma(t_[:, :], MODS[l][m_].ap, [MODS[l][m_]], [t_], t_.sem)
                K.dma(wq[:, :].rearrange("p (k n) -> p k n", n=2048), w_q[l].rearrange("(k p) n -> p k n", p=128), [], [wq], wq.sem)
                K.dma(skl[:, :].rearrange("p (a k) -> p a k", k=128), subk[l].rearrange("a n k -> n a k"), [], [skl], skl.sem)
                if final:
                    K.dma(fg[:, :], final_g.partition_broadcast(128), [], [fg], fg.sem)
                for a in range(2):
                    K.tr(bank[0][:, a * 128:(a + 1) * 128], skl[:, a * 128:(a + 1) * 128], ident[:, :], [skl, ident], [bank[0]])
                K.acopy(skT[:, :], bank[0][:, 0:256], [bank[0]], [skT])

                def routing(ti):
                    e = tiles[ti]
                    xt = xts[ti % 3]
                    h2 = h2s[ti % 2]
                    eidx = eidxs[ti % 2]
                    gate = gates[ti % 2]
                    K.norm_mod(xt, h2, SC2, SH2, S3, ss, rstd)
                    K.acopy(h2bs[ti % 2][:, :], h2[:, :], [h2], [h2bs[ti % 2]])
                    yield
                    h2T = S0
                    K.transpose8(h2, h2T, ident, [bank[0], bank[1]])
                    yield
                    qT = S1
                    for c in range(16):
                        bk = bank[2 + (c // 4) % 2]
                        for k in range(8):
                            K.mm(bk[:, (c % 4) * 128:(c % 4 + 1) * 128], wq[:, k * 2048 + c * 128:k * 2048 + (c + 1) * 128],
                                 h2T[:, k * 128:(k + 1) * 128], k == 0, k == 7, [wq, h2T], [bk])
                            if k % 2 == 1:
                                if k == 7 and c % 4 == 3:
                                    K.acopy(qT[:, (c // 4) * 512:(c // 4 + 1) * 512], bk[:, :], [bk], [qT])
                                yield
                    sc = S0
                    sbanks = [bank[0], bank[1], bank[0], bank[1]]
                    for c in range(16):
                        bk = sbanks[c // 4]
                        K.mm(bk[:, (c % 4) * 128:(c % 4 + 1) * 128], qT[:, c * 128:(c + 1) * 128], skT[:, (c % 2) * 128:(c % 2 + 1) * 128],
                             True, True, [qT, skT], [bk])
                        if c % 4 == 3:
                            K.acopy(sc[:, (c // 4) * 512:(c // 4 + 1) * 512], bk[:, :], [bk], [sc])
                            yield
                    work = S2
                    for c in range(16):
                        cs = slice(c * 128, (c + 1) * 128)
                        t0 = slice(c * 16, c * 16 + 8)
                        t1 = slice(c * 16 + 8, c * 16 + 16)
                        K.vmax(top[:, t0], sc[:, cs], [sc], [top])
                        K.vmaxidx(tidx[:, t0], top[:, t0], sc[:, cs], [sc, top], [tidx])
                        K.vmr(work[:, cs], top[:, t0], sc[:, cs], [sc, top], [work])
                        yield
                        K.vmax(top[:, t1], work[:, cs], [work], [top])
                        K.vmaxidx(tidx[:, t1], top[:, t1], work[:, cs], [work, top], [tidx])
                        yield
                    cand = S1
                    tv = top[:, :].rearrange("p (h two a) -> p h two a", two=2, a=16)
                    K.tt(cand[:, :].rearrange("p (h a b) -> p h a b", a=16, b=16),
                         tv[:, :, 0, :].unsqueeze(3).to_broadcast([128, 8, 16, 16]),
                         tv[:, :, 1, :].unsqueeze(2).to_broadcast([128, 8, 16, 16]), ALU.add, [top], [cand])
                    yield
                    cwork = S2
                    for hh in range(8):
                        cs = slice(hh * 256, (hh + 1) * 256)
                        t0 = slice(hh * 16, hh * 16 + 8)
                        t1 = slice(hh * 16 + 8, hh * 16 + 16)
                        K.vmax(best[:, t0], cand[:, cs], [cand], [best])
                        K.vmaxidx(pos[:, t0], best[:, t0], cand[:, cs], [cand, best], [pos])
                        K.vmr(cwork[:, cs], best[:, t0], cand[:, cs], [cand, best], [cwork])
                        yield
                        K.vmax(best[:, t1], cwork[:, cs], [cwork], [best])
                        K.vmaxidx(pos[:, t1], best[:, t1], cwork[:, cs], [cwork, best], [pos])
                        yield
                    P.op("dve", lambda e_: e_.tensor_single_scalar(out=pa[:, :], in_=pos[:, :], scalar=4, op=ALU.logical_shift_right), [pos], [pa])
                    P.op("dve", lambda e_: e_.tensor_single_scalar(out=pb[:, :], in_=pos[:, :], scalar=15, op=ALU.bitwise_and), [pos], [pb])
                    K.cp(tidxf[:, :], tidx[:, :], [tidx], [tidxf])
                    yield
                    oh = S3
                    tf = tidxf[:, :].rearrange("p (h two a) -> p h two a", two=2, a=16)
                    for which, (pp, sel) in enumerate(((pa, sel1), (pb, sel2))):
                        K.tt(oh[:, :].rearrange("p (h k a) -> p h k a", k=16, a=16),
                             pp[:, :].rearrange("p (h k) -> p h k", k=16).unsqueeze(3).to_broadcast([128, 8, 16, 16]),
                             iota16[:, :].unsqueeze(1).unsqueeze(1).to_broadcast([128, 8, 16, 16]), ALU.is_equal, [pp, iota16], [oh])
                        yield
                        K.tt(oh[:, :].rearrange("p (h k a) -> p h k a", k=16, a=16),
                             oh[:, :].rearrange("p (h k a) -> p h k a", k=16, a=16),
                             tf[:, :, which, :].unsqueeze(2).to_broadcast([128, 8, 16, 16]), ALU.mult, [oh, tidxf], [oh])
                        yield
                        K.red(sel[:, :], oh[:, :].rearrange("p (n a) -> p n a", a=16), ALU.add, [oh], [sel])
                        yield
                    K.stt(sel1[:, :], sel1[:, :], 128.0, sel2[:, :], ALU.mult, ALU.add, [sel1, sel2], [sel1])
                    K.cp(eidx[:, :], sel1[:, :], [sel1], [eidx])
                    b3 = best[:, :].rearrange("p (h k) -> p h k", k=16)
                    K.tt(gate[:, :].rearrange("p (h k) -> p h k", k=16), b3, b3[:, :, 0:1].to_broadcast([128, 8, 16]), ALU.subtract, [best], [gate])
                    K.act(gate[:, :], gate[:, :], AF.Exp, [gate], [gate])
                    K.red(gsum[:, :], gate[:, :].rearrange("p (h k) -> p h k", k=16), ALU.add, [gate], [gsum])
                    K.recip(gsum[:, :], gsum[:, :], [gsum], [gsum])
                    K.tt(gate[:, :].rearrange("p (h k) -> p h k", k=16), gate[:, :].rearrange("p (h k) -> p h k", k=16),
                         gsum[:, :].unsqueeze(2).to_broadcast([128, 8, 16]), ALU.mult, [gate, gsum], [gate])
                    yield

                def load_x(ti):
                    if ti < len(tiles):
                        xt_ = xts[ti % 3]
                        K.dma(xt_[:, :], src[tiles[ti]].ap, [src[tiles[ti]]], [xt_], xt_.sem)

                def accbanks(ti):
                    return [bank[4], bank[5]] if ti % 2 else [bank[6], bank[7]]

                def epilogue(ti):
                    e = tiles[ti]
                    xt = xts[ti % 3]
                    accb = accbanks(ti)
                    for n in range(2):
                        cs = slice(n * 512, (n + 1) * 512)
                        K.tt(tmp[:, cs], accb[n][:, :], G2[:, cs], ALU.mult, [accb[n], G2], [tmp])
                    K.tt(xt[:, :], tmp[:, :], xt[:, :], ALU.add, [tmp, xt], [xt])
                    if not final:
                        K.dma(dst[e].ap, xt[:, :], [xt], [dst[e]], xt.sem)
                    else:
                        K.rstd_of(xt, tmp, ss2, rstd2)
                        K.stt(tmp[:, :], xt[:, :], rstd2[:, 0:1], fg[:, :], ALU.mult, ALU.mult, [xt, rstd2, fg], [tmp])
                        K.dma(dst[e].ap, tmp[:, :], [tmp], [dst[e]], tmp.sem)

                def consume(ti, nxt):
                    e = tiles[ti]
                    h2b = h2bs[ti % 2]
                    eidx = eidxs[ti % 2]
                    gate = gates[ti % 2]
                    accb = accbanks(ti)

                    def fin(j):
                        rw = rows[j % NR]
                        dg = diags[j % 4]
                        K.act(dg[:, :], identb[:, :], AF.Copy, [identb, gdb[j]], [dg], scale=coef[:, j:j + 1])
                        for n in range(2):
                            K.mm(accb[n][:, :], dg[:, :], rw[:, 1024 + n * 512:1024 + (n + 1) * 512], j == 0, j == 127, [dg, rw], [accb[n]])

                    for j in range(128):
                        rw = rows[j % NR]
                        K.gather(rw[:, :], tabbf[l], eidx[:, j:j + 1], [eidx, TABBF[l]], [rw], rw.sem)
                        K.ttr(tmpb[:, :], rw[:, 0:1024], h2b[:, :], dd[:, j:j + 1], [rw, h2b], [tmpb, ddb[j]])
                        K.act(gd[:, j:j + 1], dd[:, j:j + 1], GELU, [ddb[j]], [gdb[j]])
                        K.act(coef[:, j:j + 1], gd[:, j:j + 1], AF.Copy, [gdb[j], gate], [gdb[j]], scale=gate[:, j:j + 1])
                        fin(j)
                        if nxt is not None and j >= 2:
                            if j < 72:
                                npull = 1
                            elif j < 80:
                                npull = 0
                            else:
                                npull = 2 if j % 4 == 1 else 1
                            for _ in range(npull):
                                next(nxt, None)
                        if j == 8:
                            if ti > 0:
                                epilogue(ti - 1)
                            load_x(ti + 2)
                    if nxt is not None:
                        for _ in nxt:
                            pass
                    if ti == len(tiles) - 1:
                        epilogue(ti)

                load_x(0)
                load_x(1)
                for _ in routing(0):
                    pass
                for ti in range(len(tiles)):
                    nxt = routing(ti + 1) if ti + 1 < len(tiles) else None
                    consume(ti, nxt)
                P.end_phase()

        def phase_mixer1():
            with ExitStack() as st:
                SH1 = T(K, st, "SH1", [128, D]); SC1 = T(K, st, "SC1", [128, D]); G1 = T(K, st, "G1", [128, D])
                BT = T(K, st, "BT", [128, 36 * 128])
                wp = T(K, st, "wp", [128, 8 * 256])
                xts = [T(K, st, "xt%d" % i, [128, D]) for i in range(4)]
                hs = [T(K, st, "h%d" % i, [128, D]) for i in range(4)]
                ss = T(K, st, "ss", [128, 1]); rstd = T(K, st, "rstd", [128, 1])
                pT = T(K, st, "pT", [128, D])
                tmp = T(K, st, "tmp", [128, 512])
                for (t_, m_) in ((SH1, 0), (SC1, 1), (G1, 2)):
                    K.dma(t_[:, :], MODS[1][m_].ap, [MODS[1][m_]], [t_], t_.sem)
                K.dma(BT[:, :].rearrange("p (m t) -> p m t", t=128), poolBT, [], [BT], BT.sem)
                K.dma(wp[:, :].rearrange("p (g i e) -> p g i e", g=4, i=2), pool_w.rearrange("g (i c) e -> c g i e", i=2), [], [wp], wp.sem)

                def stA(e):
                    xt = xts[e % 4]
                    K.dma(xt[:, :], X1[e].ap, [X1[e]], [xt], xt.sem)
                    K.norm_mod(xt, hs[e % 4], SC1, SH1, hs[e % 4], ss, rstd)

                def stB(j):
                    e = j + 2
                    kind = 0 if j == 0 else (2 if j == NOWN - 1 else 1)
                    xt = xts[e % 4]
                    for cc in range(8):
                        g = cc // 2
                        bk = bank[cc // 4]
                        for s in range(3):
                            hsrc = hs[(e - 1 + s) % 4]
                            m = (kind * 3 + s) * 4 + g
                            K.mm(bk[:, (cc % 4) * 128:(cc % 4 + 1) * 128], hsrc[:, cc * 128:(cc + 1) * 128], BT[:, m * 128:(m + 1) * 128],
                                 s == 0, s == 2, [hsrc, BT], [bk])
                        if cc % 4 == 3:
                            K.acopy(pT[:, (cc // 4) * 512:(cc // 4 + 1) * 512], bk[:, :], [bk], [pT])
                    for g in range(4):
                        yb = bank[2 + g // 2]
                        for i in range(2):
                            K.mm(yb[:, (g % 2) * 256:(g % 2 + 1) * 256], pT[:, (2 * g + i) * 128:(2 * g + i + 1) * 128],
                                 wp[:, (g * 2 + i) * 256:(g * 2 + i + 1) * 256], i == 0, i == 1, [pT, wp], [yb])
                    for n in range(2):
                        yb = bank[2 + n]
                        cs = slice(n * 512, (n + 1) * 512)
                        K.tt(tmp[:, :], yb[:, :], G1[:, cs], ALU.mult, [yb, G1], [tmp])
                        K.tt(xt[:, cs], tmp[:, :], xt[:, cs], ALU.add, [tmp, xt], [xt])
                    K.dma(XMID1[j].ap, xt[:, :], [xt], [XMID1[j]], xt.sem)

                for e in range(1, NEXT - 1):
                    stA(e)
                    if e >= 3:
                        stB(e - 3)
                P.end_phase()

        conv_jobs = []

        def mk_conv(l, i, cs_):
            return lambda: K.dma(tabbf[l][i * 512:(i + 1) * 512, :], tab[l][i * 512:(i + 1) * 512, :], [], [TABBF[l]], cs_, eng="pool")

        for l in range(2):
            cs_ = Sem(G.enter_context(nc.semaphore("conv%d" % l)), "conv%d" % l)
            for i in range(32):
                conv_jobs.append(mk_conv(l, i, cs_))
        phase_mods(0)
        phase_ctx()
        phase_mixer0()
        phase_peer(0, list(range(1, NEXT - 1)), XMID0, X1, False)
        phase_mods(1)
        phase_mixer1()
        own_src = {j: XMID1[j] for j in range(NOWN)}
        phase_peer(1, list(range(NOWN)), own_src, OUT, True)
    return nc


def _rope_table(pos):
    rowp = (pos // 64).astype(np.float32)
    colp = (pos % 64).astype(np.float32)
    inv = np.power(np.float32(10000.0), -np.arange(16, dtype=np.float32) / np.float32(16)).astype(np.float32)
    ar = rowp[:, None] * inv[None, :]
    ac = colp[:, None] * inv[None, :]
    cr, sr, cc, sc = np.cos(ar), np.sin(ar), np.cos(ac), np.sin(ac)
    cos64 = np.concatenate([cr, cr, cc, cc], axis=1)
    sin64 = np.concatenate([-sr, sr, -sc, sc], axis=1)
    return np.concatenate([cos64, sin64], axis=1).astype(np.float32)


def _pool_BT(base, L):
    out = np.zeros((3, 4, 128, 128), np.float32)
    for g, w in enumerate((2, 4, 8, 16)):
        for t in range(128):
            tp = base + t
            lo = max(tp - w // 2, 0)
            hi = min(tp + w // 2, L)
            cnt = hi - lo
            for src in range(lo, hi):
                rel = src - (base - 128)
                s, r = rel // 128, rel % 128
                out[s, g, r, t] += 1.0 / cnt
            out[1, g, t, t] -= 1.0
    return out


def _prep(inputs):
    f = lambda a: np.ascontiguousarray(np.asarray(a, dtype=np.float32))
    x = f(inputs["x"]); c = f(inputs["c"]); ctx = f(inputs["ctx"]); c_ctx = f(inputs["c_ctx"])
    L = x.shape[1]
    shared = {
        "mod_w": f(inputs["mod_w"]), "mod_b": f(inputs["mod_b"]),
        "norm1_g": f(inputs["norm1_g"]), "norm2_g": f(inputs["norm2_g"]),
        "attn_in_w": f(inputs["attn_in_w"][0]), "attn_out_w": f(inputs["attn_out_w"][0]),
        "attn_sink": f(inputs["attn_sink"][0]), "gmlp_w_s": f(inputs["gmlp_w_s"][0]),
        "bs_col": f(np.asarray(inputs["gmlp_b_s"][0]).T),
        "pool_w": f(inputs["pool_w"][0]), "pool_scale": f(inputs["pool_scale"][0]),
        "peer_w_q": f(inputs["peer_w_q"]), "peer_sub_keys": f(inputs["peer_sub_keys"]),
        "peer_tab0": np.ascontiguousarray(np.concatenate([np.asarray(inputs["peer_down"][0], np.float32), np.asarray(inputs["peer_up"][0], np.float32)], axis=1)),
        "peer_tab1": np.ascontiguousarray(np.concatenate([np.asarray(inputs["peer_down"][1], np.float32), np.asarray(inputs["peer_up"][1], np.float32)], axis=1)),
        "final_g": f(inputs["final_g"]),
        "ident": np.eye(128, dtype=np.float32),
        "iota16": np.tile(np.arange(16, dtype=np.float32)[None, :], (128, 1)),
    }
    jj = np.arange(128)[:, None]
    ii = np.arange(128)[None, :]
    masks = np.stack([(ii <= jj), (jj <= ii)], axis=1).astype(np.float32)
    shared["masks"] = np.ascontiguousarray(masks)
    maps = []
    for core in range(NCORE):
        b, q = core // 4, core % 4
        start = q * OWN
        lo = start - 256
        xe = np.zeros((NEXT * 128, D), np.float32)
        s0, s1 = max(lo, 0), min(lo + NEXT * 128, L)
        xe[s0 - lo:s1 - lo] = x[b, s0:s1]
        pos = np.clip(np.arange(lo, lo + NEXT * 128), 0, L - 1)
        kv = np.array([1.0 if 0 <= lo + e * 128 < L else 0.0 for e in range(NEXT)], np.float32)
        BT = np.zeros((3, 3, 4, 128, 128), np.float32)
        for kind, j in enumerate((0, 1, NOWN - 1)):
            BT[kind] = _pool_BT(start + j * 128, L)
        BTl = np.ascontiguousarray(BT.reshape(36, 128, 128).transpose(1, 0, 2))
        m = dict(shared)
        m.update({
            "x_ext": xe,
            "ctx_b": np.ascontiguousarray(ctx[b]),
            "c_cols": np.ascontiguousarray(np.concatenate([c[b].reshape(8, 128).T, c_ctx.reshape(8, 128).T], axis=1)),
            "rope": _rope_table(pos),
            "kvalid": np.ascontiguousarray(np.tile(kv[None, :], (128, 1))),
            "poolBT": BTl,
        })
        maps.append(m)
    return maps


_NC_CACHE = {}


def kernel(**inputs):
    maps = _prep(inputs)
    if "nc" not in _NC_CACHE:
        _NC_CACHE["nc"] = build_nc(False)
    nc = _NC_CACHE["nc"]
    res = run_bass_kernel_spmd(nc, maps, core_ids=list(range(NCORE)))
    B = 2
    out = np.zeros((B, 4 * OWN, D), np.float32)
    for core in range(NCORE):
        b, q = core // 4, core % 4
        out[b, q * OWN:(q + 1) * OWN] = np.asarray(res.results[core]["out"], np.float32)
    return out
```

```python
import numpy as np
from contextlib import ExitStack
import concourse.bass as bass
import concourse.mybir as mybir
from concourse.bass_utils import run_bass_kernel_spmd

F32 = mybir.dt.float32
U32 = mybir.dt.uint32
BF16 = mybir.dt.bfloat16
AF = mybir.ActivationFunctionType
ALU = mybir.AluOpType
AX = mybir.AxisListType

D = 1024
NCORE = 8
OWN = 4096
NOWN = 32
NEXT = 36
EPS = 1e-6
GELU = AF.Gelu_apprx_tanh
NEG = -1e30


class Sem:
    __slots__ = ("h", "val", "name")

    def __init__(self, h, name):
        self.h = h
        self.val = 0
        self.name = name


class Buf:
    __slots__ = ("name", "w", "r")

    def __init__(self, name):
        self.name = name
        self.w = None
        self.r = {}


def _b(x):
    return x if isinstance(x, Buf) else x.b


ENGS = ("pe", "act", "dve", "pool", "sp")


class Prog:
    def __init__(self, nc, stack, n_dsem=64):
        self.nc = nc
        self.q = {e: [] for e in ENGS}
        self.esem = {}
        for e in ENGS:
            if e != "sp":
                self.esem[e] = Sem(stack.enter_context(nc.semaphore("e_" + e)), e)
        self.dpool = [Sem(stack.enter_context(nc.semaphore("d%d" % i)), "d%d" % i) for i in range(n_dsem)]
        self.dfree = list(self.dpool)
        self.seen = {e: {} for e in ENGS}
        self.nblock = 0

    def get_dsem(self):
        return self.dfree.pop()

    def op(self, eng, fn, reads=(), writes=(), dsem=None):
        need = {}
        for x in reads:
            b = _b(x)
            if b.w is not None:
                s, v = b.w
                if need.get(s, 0) < v:
                    need[s] = v
        for x in writes:
            b = _b(x)
            if b.w is not None:
                s, v = b.w
                if need.get(s, 0) < v:
                    need[s] = v
            for s, v in b.r.items():
                if need.get(s, 0) < v:
                    need[s] = v
        own = self.esem.get(eng)
        seen = self.seen[eng]
        q = self.q[eng]
        for s, v in need.items():
            if eng == "pe" and s is own:
                continue
            if seen.get(s, 0) >= v:
                continue
            seen[s] = v
            q.append(("w", s, v))
        if dsem is not None:
            sem = dsem
            sem.val += 16
            inc = 16
        else:
            sem = own
            sem.val += 1
            inc = 1
        q.append(("o", fn, sem, inc))
        for x in reads:
            b = _b(x)
            if b.r.get(sem, 0) < sem.val:
                b.r[sem] = sem.val
        tag = (sem, sem.val)
        for x in writes:
            b = _b(x)
            b.w = tag
            b.r = {}

    def end_phase(self):
        allsems = list(self.esem.values()) + self.dpool
        for e in ENGS:
            seen = self.seen[e]
            for s in allsems:
                if s.val > 0 and seen.get(s, 0) < s.val:
                    seen[s] = s.val
                    self.q[e].append(("w", s, s.val))
        nc = self.nc
        q = self.q
        with nc.Block() as block:
            def run(engine, items):
                for it in items:
                    if it[0] == "w":
                        engine.wait_ge(it[1].h, it[2])
                    else:
                        it[1](engine).then_inc(it[2].h, it[3])

            @block.tensor
            def _(e):
                run(e, q["pe"])

            @block.scalar
            def _(e):
                run(e, q["act"])

            @block.vector
            def _(e):
                run(e, q["dve"])

            @block.gpsimd
            def _(e):
                run(e, q["pool"])

            @block.sync
            def _(e):
                run(e, q["sp"])
        self.q = {e: [] for e in ENGS}
        self.dfree = list(self.dpool)
        self.nblock += 1


class T:
    def __init__(self, K, st, name, shape, dt=F32, psum=False):
        K.uid += 1
        nm = "%s_%d" % (name, K.uid)
        alloc = K.nc.psum_tensor if psum else K.nc.sbuf_tensor
        self.t = st.enter_context(alloc(nm, shape, dt))
        self.b = Buf(nm)
        self.K = K
        self._sem = None

    def __getitem__(self, k):
        return self.t[k]

    @property
    def sem(self):
        if self._sem is None:
            self._sem = self.K.P.get_dsem()
        return self._sem


class Dr:
    def __init__(self, ap, name):
        self.ap = ap
        self.b = Buf(name)


class Kern:
    def __init__(self, nc, gst):
        self.nc = nc
        self.uid = 0
        self.P = Prog(nc, gst)

    def dma(self, out, in_, reads, writes, sem, eng="sp"):
        self.P.op(eng, lambda e: e.dma_start(out=out, in_=in_), reads, writes, dsem=sem)

    def mm(self, out, lhsT, rhs, start, stop, reads, writes, skip=False):
        self.P.op("pe", lambda e: e.matmul(out, lhsT=lhsT, rhs=rhs, start=start, stop=stop, skip_group_check=skip), reads, writes)

    def tr(self, out, in_, ident, reads, writes):
        self.P.op("pe", lambda e: e.transpose(out=out, in_=in_, identity=ident), reads, writes)

    def act(self, out, in_, func, reads, writes, **kw):
        self.P.op("act", lambda e: e.activation(out=out, in_=in_, func=func, **kw), reads, writes)

    def acopy(self, out, in_, reads, writes):
        self.P.op("act", lambda e: e.copy(out=out, in_=in_), reads, writes)

    def tt(self, out, in0, in1, op, reads, writes, eng="dve"):
        self.P.op(eng, lambda e: e.tensor_tensor(out=out, in0=in0, in1=in1, op=op), reads, writes)

    def ts(self, out, in0, s1, s2, op0, op1, reads, writes, eng="dve"):
        if op1 is None:
            self.P.op(eng, lambda e: e.tensor_scalar(out=out, in0=in0, scalar1=s1, scalar2=None, op0=op0), reads, writes)
        else:
            self.P.op(eng, lambda e: e.tensor_scalar(out=out, in0=in0, scalar1=s1, scalar2=s2, op0=op0, op1=op1), reads, writes)

    def stt(self, out, in0, scalar, in1, op0, op1, reads, writes):
        self.P.op("dve", lambda e: e.scalar_tensor_tensor(out=out, in0=in0, scalar=scalar, in1=in1, op0=op0, op1=op1), reads, writes)

    def cp(self, out, in_, reads, writes, eng="dve"):
        self.P.op(eng, lambda e: e.tensor_copy(out=out, in_=in_), reads, writes)

    def recip(self, out, in_, reads, writes):
        self.P.op("dve", lambda e: e.reciprocal(out=out, in_=in_), reads, writes)

    def memset(self, out, val, writes, eng="pool"):
        self.P.op(eng, lambda e: e.memset(out, val), (), writes)

    def red(self, out, in_, op, reads, writes):
        self.P.op("dve", lambda e: e.tensor_reduce(out=out, in_=in_, axis=AX.X, op=op), reads, writes)

    def ttr(self, out, in0, in1, accum, reads, writes):
        self.P.op("dve", lambda e: e.scalar_tensor_tensor(out=out, in0=in0, scalar=1.0, in1=in1, op0=ALU.mult, op1=ALU.mult,
                                                         accum_out=accum), reads, writes)

    def vmax(self, out, in_, reads, writes):
        self.P.op("dve", lambda e: e.max(out=out, in_=in_), reads, writes)

    def vmaxidx(self, out, in_max, in_values, reads, writes):
        self.P.op("dve", lambda e: e.max_index(out=out, in_max=in_max, in_values=in_values), reads, writes)

    def vmr(self, out, rep, vals, reads, writes):
        self.P.op("dve", lambda e: e.match_replace(out=out, in_to_replace=rep, in_values=vals, imm_value=NEG), reads, writes)

    def gather(self, out, tab, idx, reads, writes, sem):
        self.P.op("pool", lambda e: e.indirect_dma_start(out=out, out_offset=None, in_=tab,
                                                         in_offset=bass.IndirectOffsetOnAxis(ap=idx, axis=0)),
                  reads, writes, dsem=sem)

    def rstd_of(self, xt, junk, ss, rstd):
        self.act(junk[:, 0:D], xt[:, :], AF.Square, [xt], [junk, ss], accum_out=ss[:, 0:1])
        self.act(rstd[:, 0:1], ss[:, 0:1], AF.Sqrt, [ss], [rstd], scale=1.0 / D, bias=EPS)
        self.recip(rstd[:, 0:1], rstd[:, 0:1], [rstd], [rstd])

    def norm_mod(self, xt, h, SC, SH, junk, ss, rstd):
        self.rstd_of(xt, junk, ss, rstd)
        self.stt(h[:, :], xt[:, :], rstd[:, 0:1], SC[:, :], ALU.mult, ALU.mult, [xt, rstd, SC], [h])
        self.tt(h[:, :], h[:, :], SH[:, :], ALU.add, [h, SH], [h])

    def transpose8(self, src, dst, ident, banks):
        for half in range(2):
            bk = banks[half]
            for k in range(4):
                kk = half * 4 + k
                self.tr(bk[:, k * 128:(k + 1) * 128], src[:, kk * 128:(kk + 1) * 128], ident[:, :], [src, ident], [bk])
            self.acopy(dst[:, half * 512:(half + 1) * 512], bk[:, :], [bk], [dst])


def build_nc(debug=False):
    nc = bass.Bass("TRN2", target_bir_lowering=False)

    def din(name, shape, dt=F32):
        return nc.dram_tensor(name, shape, dt, kind="ExternalInput").ap()

    skind = "ExternalOutput" if debug else "Internal"

    def dscr(name, shape):
        return nc.dram_tensor(name, shape, F32, kind=skind).ap()

    x_ext = din("x_ext", [NEXT * 128, D])
    ctx_b = din("ctx_b", [256, D])
    c_cols = din("c_cols", [128, 16])
    rope = din("rope", [NEXT * 128, 128])
    kvalid_d = din("kvalid", [128, NEXT])
    poolBT = din("poolBT", [128, 36, 128])
    ident_d = din("ident", [128, 128])
    masks_d = din("masks", [128, 2, 128])
    iota_d = din("iota16", [128, 16])
    mod_w = din("mod_w", [2, D, 6 * D])
    mod_b = din("mod_b", [2, 6 * D])
    norm1_g = din("norm1_g", [2, D])
    norm2_g = din("norm2_g", [2, D])
    w_in = din("attn_in_w", [D, 1792])
    w_out = din("attn_out_w", [D, D])
    sink_d = din("attn_sink", [8])
    w_s = din("gmlp_w_s", [8, 128, 128])
    bs_col = din("bs_col", [128, 8])
    pool_w = din("pool_w", [4, 256, 256])
    pool_scale = din("pool_scale", [D])
    w_q = din("peer_w_q", [2, D, 2048])
    subk = din("peer_sub_keys", [2, 2, 128, 128])
    tab = [din("peer_tab0", [16384, 2048]), din("peer_tab1", [16384, 2048])]
    final_g = din("final_g", [D])
    out_d = nc.dram_tensor("out", [OWN, D], F32, kind="ExternalOutput").ap()

    mods_d = dscr("mods", [2, 8, 128, D])
    xmid0_d = dscr("xmid0", [NEXT * 128, D])
    x1_d = dscr("x1", [NEXT * 128, D])
    xmid1_d = dscr("xmid1", [OWN, D])
    tabbf = [nc.dram_tensor("tabbf%d" % l, [16384, 2048], BF16, kind=skind).ap() for l in range(2)]

    with ExitStack() as G:
        K = Kern(nc, G)
        P = K.P
        bank = [T(K, G, "bank%d" % i, [128, 512], F32, psum=True) for i in range(8)]
        ident = T(K, G, "ident", [128, 128])
        masks = T(K, G, "masks", [128, 256])
        iota16 = T(K, G, "iota16", [128, 16])
        kvalid = T(K, G, "kvalid", [128, NEXT])
        KcT = T(K, G, "KcT", [64, 512], BF16)
        Vc = [T(K, G, "Vc%d" % i, [128, 130], BF16) for i in range(2)]

        MODS = [[Dr(mods_d[l, i], "mods%d_%d" % (l, i)) for i in range(8)] for l in range(2)]
        XMID0 = [Dr(xmid0_d[e * 128:(e + 1) * 128, :], "xmid0_%d" % e) for e in range(NEXT)]
        X1 = [Dr(x1_d[e * 128:(e + 1) * 128, :], "x1_%d" % e) for e in range(NEXT)]
        XMID1 = [Dr(xmid1_d[j * 128:(j + 1) * 128, :], "xmid1_%d" % j) for j in range(NOWN)]
        OUT = [Dr(out_d[j * 128:(j + 1) * 128, :], "out_%d" % j) for j in range(NOWN)]
        TABBF = [Buf("tabbf0"), Buf("tabbf1")]

        K.dma(ident[:, :], ident_d, [], [ident], ident.sem)
        K.dma(masks[:, :], masks_d.rearrange("p a b -> p (a b)"), [], [masks], masks.sem)
        K.dma(iota16[:, :], iota_d, [], [iota16], iota16.sem)
        K.dma(kvalid[:, :], kvalid_d, [], [kvalid], kvalid.sem)

        def phase_mods(l):
            with ExitStack() as st:
                cc = T(K, st, "cc", [128, 16])
                sil = T(K, st, "sil", [128, 16])
                cb = T(K, st, "cb", [128, 16 * 128])
                modb = T(K, st, "modb", [128, 6 * D])
                gb1 = T(K, st, "gb1", [128, D])
                gb2 = T(K, st, "gb2", [128, D])
                psb = T(K, st, "psb", [128, D])
                wsl = [T(K, st, "wsl%d" % i, [128, 2048]) for i in range(3)]
                res = [T(K, st, "res%d" % i, [128, 512]) for i in range(4)]
                K.dma(cc[:, :], c_cols, [], [cc], cc.sem)
                K.dma(modb[:, :], mod_b[l, :].partition_broadcast(128), [], [modb], modb.sem)
                K.dma(gb1[:, :], norm1_g[l, :].partition_broadcast(128), [], [gb1], gb1.sem)
                K.dma(gb2[:, :], norm2_g[l, :].partition_broadcast(128), [], [gb2], gb2.sem)
                if l == 1:
                    K.dma(psb[:, :], pool_scale.partition_broadcast(128), [], [psb], psb.sem)
                K.act(sil[:, :], cc[:, :], AF.Silu, [cc], [sil])
                K.cp(cb[:, :].rearrange("p (k m) -> p k m", m=128), sil[:, :].unsqueeze(2).to_broadcast([128, 16, 128]), [sil], [cb])
                nload = 0
                nres = 0
                for p in range(3):
                    do_ctx = (l == 0 and p == 0)
                    for k in range(8):
                        ws = wsl[nload % 3]
                        nload += 1
                        K.dma(ws[:, :], mod_w[l, k * 128:(k + 1) * 128, p * 2048:(p + 1) * 2048], [], [ws], ws.sem)
                        for j in range(4):
                            K.mm(bank[j][:, :], cb[:, k * 128:(k + 1) * 128], ws[:, j * 512:(j + 1) * 512], k == 0, k == 7, [cb, ws], [bank[j]])
                        if do_ctx:
                            for j in range(4):
                                K.mm(bank[4 + j][:, :], cb[:, (8 + k) * 128:(9 + k) * 128], ws[:, j * 512:(j + 1) * 512], k == 0, k == 7, [cb, ws], [bank[4 + j]])
                    for isctx in ([False, True] if do_ctx else [False]):
                        for j in range(4):
                            cbk = p * 4 + j
                            mi = cbk // 2
                            half = cbk % 2
                            hs = slice(half * 512, (half + 1) * 512)
                            bk = bank[4 + j] if isctx else bank[j]
                            r = res[nres % 4]
                            nres += 1
                            K.tt(r[:, :], bk[:, :], modb[:, cbk * 512:(cbk + 1) * 512], ALU.add, [bk, modb], [r])
                            if mi in (1, 4):
                                gb = gb1 if mi == 1 else gb2
                                K.stt(r[:, :], r[:, :], 1.0, gb[:, hs], ALU.add, ALU.mult, [r, gb], [r])
                            if l == 1 and mi == 2:
                                K.tt(r[:, :], r[:, :], psb[:, hs], ALU.mult, [r, psb], [r])
                            dst = MODS[l][6 + mi] if isctx else MODS[l][mi]
                            K.dma(dst.ap[:, hs], r[:, :], [r], [dst], r.sem)
                P.end_phase()

        def phase_ctx():
            with ExitStack() as st:
                CSH = T(K, st, "CSH", [128, D])
                CSC = T(K, st, "CSC", [128, D])
                wkv = T(K, st, "wkv", [128, 8 * 256], BF16)
                xt = T(K, st, "xt", [128, D])
                h = T(K, st, "h", [128, D])
                hT = T(K, st, "hT", [128, D], BF16)
                junk = h
                ss = T(K, st, "ss", [128, 1])
                rstd = T(K, st, "rstd", [128, 1])
                ktmp = T(K, st, "ktmp", [128, 128])
                K.dma(CSH[:, :], MODS[0][6].ap, [MODS[0][6]], [CSH], CSH.sem)
                K.dma(CSC[:, :], MODS[0][7].ap, [MODS[0][7]], [CSC], CSC.sem)
                K.dma(wkv[:, :].rearrange("p (k n) -> p k n", n=256), w_in.rearrange("(k p) n -> p k n", p=128)[:, :, 512:768], [], [wkv], wkv.sem, eng="pool")
                for ct in range(2):
                    K.memset(Vc[ct][:, :], 1.0, [Vc[ct]])
                for ct in range(2):
                    K.dma(xt[:, :], ctx_b[ct * 128:(ct + 1) * 128, :], [], [xt], xt.sem)
                    K.norm_mod(xt, h, CSC, CSH, junk, ss, rstd)
                    K.transpose8(h, hT, ident, [bank[0], bank[1]])
                    for k in range(8):
                        K.mm(bank[2][:, 0:256], hT[:, k * 128:(k + 1) * 128], wkv[:, k * 256:(k + 1) * 256], k == 0, k == 7, [hT, wkv], [bank[2]])
                    K.cp(Vc[ct][:, :].rearrange("p (g d) -> p g d", d=65)[:, :, 0:64],
                         bank[2][:, 128:256].rearrange("p (g d) -> p g d", d=64), [bank[2]], [Vc[ct]])
                    K.acopy(ktmp[:, :], bank[2][:, 0:128], [bank[2]], [ktmp])
                    for g in range(2):
                        K.tr(bank[3][0:64, g * 128:(g + 1) * 128], ktmp[:, g * 64:(g + 1) * 64], ident[:, :], [ktmp, ident], [bank[3]])
                    K.cp(KcT[:, :].rearrange("p (g c j) -> p g c j", g=2, c=2)[:, :, ct, :],
                         bank[3][0:64, 0:256].rearrange("p (g j) -> p g j", g=2), [bank[3]], [KcT])
                P.end_phase()

        def phase_mixer0():
            with ExitStack() as st:
                SH1 = T(K, st, "SH1", [128, D]); SC1 = T(K, st, "SC1", [128, D]); G1 = T(K, st, "G1", [128, D])
                win = T(K, st, "win", [128, 8 * 1792], BF16)
                wout = T(K, st, "wout", [128, 8 * 1024], BF16)
                wsT = T(K, st, "wsT", [128, 8 * 128], BF16)
                bsc = T(K, st, "bsc", [128, 8])
                esink = T(K, st, "esink", [128, 8])
                xts = [T(K, st, "xt%d" % i, [128, D]) for i in range(3)]
                rps = [T(K, st, "rp%d" % i, [128, 128]) for i in range(2)]
                h = T(K, st, "h", [128, D])
                hT = T(K, st, "hT", [128, D], BF16)
                junk = h
                ss = T(K, st, "ss", [128, 1]); rstd = T(K, st, "rstd", [128, 1])
                qr = T(K, st, "qr", [128, 512]); qs = T(K, st, "qs", [128, 512])
                kr = T(K, st, "kr", [128, 128]); ks = T(K, st, "ks", [128, 128])
                QT = [T(K, st, "QT%d" % i, [64, 1024], BF16) for i in range(3)]
                KT = [T(K, st, "KT%d" % i, [64, 256], BF16) for i in range(4)]
                Vr = [T(K, st, "Vr%d" % i, [128, 130], BF16) for i in range(4)]
                U = [T(K, st, "U%d" % i, [128, 512]) for i in range(3)]
                VN = [T(K, st, "VN%d" % i, [128, 512], BF16) for i in range(3)]
                vg = T(K, st, "vg", [128, 512]); cen = T(K, st, "cen", [128, 512]); sq = vg
                mu = T(K, st, "mu", [128, 8]); var = T(K, st, "var", [128, 8])
                pts = [T(K, st, "pt%d" % i, [128, 512], BF16) for i in range(4)]
                den = T(K, st, "den", [128, 8]); rden = T(K, st, "rden", [128, 8])
                cat = T(K, st, "cat", [128, D]); catT = T(K, st, "catT", [128, D], BF16)
                tmp = T(K, st, "tmp", [128, 512])
                wsl = cat

                for (t_, m_) in ((SH1, 0), (SC1, 1), (G1, 2)):
                    K.dma(t_[:, :], MODS[0][m_].ap, [MODS[0][m_]], [t_], t_.sem)
                K.dma(win[:, :].rearrange("p (k n) -> p k n", n=1792), w_in.rearrange("(k p) n -> p k n", p=128), [], [win], win.sem, eng="pool")
                K.dma(wout[:, :].rearrange("p (k n) -> p k n", n=1024), w_out.rearrange("(k p) n -> p k n", p=128), [], [wout], wout.sem, eng="pool")
                K.dma(wsl[:, :].rearrange("p (g q) -> p g q", q=128), w_s.rearrange("g p q -> p g q"), [], [wsl], wsl.sem)
                K.dma(bsc[:, :], bs_col, [], [bsc], bsc.sem)
                K.dma(esink[:, :], sink_d.partition_broadcast(128), [], [esink], esink.sem)
                K.act(esink[:, :], esink[:, :], AF.Exp, [esink], [esink])
                for g in range(8):
                    bk = bank[g // 4]
                    K.tr(bk[:, (g % 4) * 128:(g % 4 + 1) * 128], wsl[:, g * 128:(g + 1) * 128], ident[:, :], [wsl, ident], [bk])
                    if g % 4 == 3:
                        K.acopy(wsT[:, (g // 4) * 512:(g // 4 + 1) * 512], bk[:, :], [bk], [wsT])

                def do_rope(src_ap, nh, cr, cs, rp, srcb):
                    v5 = "p (h f two d) -> p h f two d"
                    K.tt(cr[:, :].rearrange("p (h d) -> p h d", d=64), src_ap.rearrange("p (h d) -> p h d", d=64),
                         rp[:, 0:64].unsqueeze(1).to_broadcast([128, nh, 64]), ALU.mult, [srcb, rp], [cr])
                    for j in range(2):
                        K.tt(cs[:, :].rearrange(v5, h=nh, f=2, two=2)[:, :, :, j, :],
                             src_ap.rearrange(v5, h=nh, f=2, two=2)[:, :, :, 1 - j, :],
                             rp[:, 64:128].rearrange("p (f two d) -> p f two d", f=2, two=2)[:, :, j, :].unsqueeze(1).to_broadcast([128, nh, 2, 16]),
                             ALU.mult, [srcb, rp], [cs])
                    K.tt(cr[:, :], cr[:, :], cs[:, :], ALU.add, [cr, cs], [cr])

                def stageA(e):
                    full = 1 <= e <= NEXT - 2
                    xt = xts[e % 3]
                    rp = rps[e % 2]
                    K.dma(xt[:, :], x_ext[e * 128:(e + 1) * 128, :], [], [xt], xt.sem)
                    K.dma(rp[:, :], rope[e * 128:(e + 1) * 128, :], [], [rp], rp.sem)
                    yield
                    K.norm_mod(xt, h, SC1, SH1, junk, ss, rstd)
                    yield
                    K.transpose8(h, hT, ident, [bank[0], bank[1]])
                    yield
                    for k in range(8):
                        lt = hT[:, k * 128:(k + 1) * 128]
                        wb = k * 1792
                        if full:
                            K.mm(bank[2][:, :], lt, win[:, wb:wb + 512], k == 0, k == 7, [hT, win], [bank[2]])
                        K.mm(bank[3][:, 0:256], lt, win[:, wb + 512:wb + 768], k == 0, k == 7, [hT, win], [bank[3]])
                        if full:
                            K.mm(bank[0][:, :], lt, win[:, wb + 768:wb + 1280], k == 0, k == 7, [hT, win], [bank[0]])
                            K.mm(bank[1][:, :], lt, win[:, wb + 1280:wb + 1792], k == 0, k == 7, [hT, win], [bank[1]])
                        if k % 2 == 1:
                            yield
                    do_rope(bank[3][:, 0:128], 2, kr, ks, rp, bank[3])
                    vr = Vr[e % 4]
                    K.ts(vr[:, :].rearrange("p (g d) -> p g d", d=65)[:, :, 0:64], bank[3][:, 128:256].rearrange("p (g d) -> p g d", d=64),
                         kvalid[:, e:e + 1], None, ALU.mult, None, [bank[3], kvalid], [vr])
                    K.cp(vr[:, :].rearrange("p (g d) -> p g d", d=65)[:, :, 64:65], kvalid[:, e:e + 1].unsqueeze(1).to_broadcast([128, 2, 1]), [kvalid, vr], [vr])
                    yield
                    kt = KT[e % 4]
                    for g in range(2):
                        K.tr(bank[3][0:64, g * 128:(g + 1) * 128], kr[:, g * 64:(g + 1) * 64], ident[:, :], [kr, ident], [bank[3]])
                    K.acopy(kt[:, :], bank[3][0:64, 0:256], [bank[3]], [kt])
                    yield
                    if not full:
                        return
                    u = U[e % 3]
                    vn = VN[e % 3]
                    K.act(u[:, :], bank[0][:, :], GELU, [bank[0]], [u])
                    K.act(vg[:, :], bank[1][:, :], GELU, [bank[1]], [vg])
                    yield
                    do_rope(bank[2][:, :], 8, qr, qs, rp, bank[2])
                    yield
                    qt = QT[e % 3]
                    for hh in range(8):
                        bk = bank[2 * (hh // 4)]
                        K.tr(bk[0:64, (hh % 4) * 128:(hh % 4 + 1) * 128], qr[:, hh * 64:(hh + 1) * 64], ident[:, :], [qr, ident], [bk])
                        if hh % 4 == 3:
                            K.acopy(qt[:, (hh // 4) * 512:(hh // 4 + 1) * 512], bk[0:64, :], [bk], [qt])
                            yield
                    g3 = "p (g d) -> p g d"
                    K.red(mu[:, :], vg[:, :].rearrange(g3, d=64), ALU.add, [vg], [mu])
                    K.ts(mu[:, :], mu[:, :], 1.0 / 64, None, ALU.mult, None, [mu], [mu])
                    K.tt(cen[:, :].rearrange(g3, d=64), vg[:, :].rearrange(g3, d=64), mu[:, :].unsqueeze(2).to_broadcast([128, 8, 64]), ALU.subtract, [vg, mu], [cen])
                    yield
                    K.tt(sq[:, :], cen[:, :], cen[:, :], ALU.mult, [cen], [sq])
                    K.red(var[:, :], sq[:, :].rearrange(g3, d=64), ALU.add, [sq], [var])
                    K.act(var[:, :], var[:, :], AF.Sqrt, [var], [var], scale=1.0 / 64, bias=EPS)
                    K.recip(var[:, :], var[:, :], [var], [var])
                    yield
                    K.tt(vn[:, :].rearrange(g3, d=64), cen[:, :].rearrange(g3, d=64), var[:, :].unsqueeze(2).to_broadcast([128, 8, 64]), ALU.mult, [cen, var], [vn])
                    yield

                npt = [0]

                def stageB(e, tick):
                    xt = xts[e % 3]
                    qt = QT[e % 3]
                    steps = []
                    for gk in range(2):
                        for ti, (kind, s_, mk) in enumerate([("w", e - 1, 0), ("w", e, None), ("w", e + 1, 1), ("c", 0, None), ("c", 1, None)]):
                            steps.append((gk, ti, kind, s_, mk))

                    def srcs(gk, kind, s_):
                        if kind == "w":
                            ksrc = KT[s_ % 4]
                            return ksrc, ksrc[:, gk * 128:(gk + 1) * 128], Vr[s_ % 4]
                        return KcT, KcT[:, gk * 256 + s_ * 128:gk * 256 + (s_ + 1) * 128], Vc[s_]

                    def score(i):
                        gk, ti, kind, s_, mk = steps[i]
                        ksrc, lhs, vsrc = srcs(gk, kind, s_)
                        sb_ = bank[4 + (i % 2)]
                        K.mm(sb_[:, :], lhs, qt[:, gk * 512:(gk + 1) * 512], True, True, [ksrc, qt], [sb_])

                    score(0)
                    for i, (gk, ti, kind, s_, mk) in enumerate(steps):
                        ksrc, lhs, vsrc = srcs(gk, kind, s_)
                        ob = bank[6 + gk]
                        sb_ = bank[4 + (i % 2)]
                        pt = pts[npt[0] % 4]
                        npt[0] += 1
                        if i + 1 < len(steps):
                            score(i + 1)
                        K.act(pt[:, :], sb_[:, :], AF.Exp, [sb_], [pt], scale=0.125)
                        if mk is not None:
                            K.tt(pt[:, :].rearrange("p (h q) -> p h q", q=128), pt[:, :].rearrange("p (h q) -> p h q", q=128),
                                 masks[:, mk * 128:(mk + 1) * 128].unsqueeze(1).to_broadcast([128, 4, 128]), ALU.mult, [pt, masks], [pt])
                        for hh in range(4):
                            K.mm(ob[:, hh * 65:(hh + 1) * 65], pt[:, hh * 128:(hh + 1) * 128], vsrc[:, gk * 65:(gk + 1) * 65],
                                 ti == 0 and hh == 0, ti == 4, [pt, vsrc], [ob], skip=True)
                        tick()
                        if ti == 4:
                            o3 = ob[:, 0:260].rearrange("p (h d) -> p h d", d=65)
                            K.tt(den[:, gk * 4:(gk + 1) * 4].unsqueeze(2), o3[:, :, 64:65], esink[:, gk * 4:(gk + 1) * 4].unsqueeze(2), ALU.add, [ob, esink], [den])
                            K.recip(rden[:, gk * 4:(gk + 1) * 4], den[:, gk * 4:(gk + 1) * 4], [den], [rden])
                            K.tt(cat[:, gk * 256:(gk + 1) * 256].rearrange("p (h d) -> p h d", d=64), o3[:, :, 0:64],
                                 rden[:, gk * 4:(gk + 1) * 4].unsqueeze(2).to_broadcast([128, 4, 64]), ALU.mult, [ob, rden], [cat])
                    vn = VN[e % 3]
                    u = U[e % 3]
                    for g in range(8):
                        K.mm(bank[4][:, g * 64:(g + 1) * 64], wsT[:, g * 128:(g + 1) * 128], vn[:, g * 64:(g + 1) * 64], True, True, [wsT, vn], [bank[4]])
                    K.tt(tmp[:, :].rearrange("p (g d) -> p g d", d=64), bank[4][:, :].rearrange("p (g d) -> p g d", d=64),
                         bsc[:, :].unsqueeze(2).to_broadcast([128, 8, 64]), ALU.add, [bank[4], bsc], [tmp])
                    K.tt(cat[:, 512:1024], tmp[:, :], u[:, :], ALU.mult, [tmp, u], [cat])
                    tick()
                    K.transpose8(cat, catT, ident, [bank[4], bank[5]])
                    tick()
                    for n in range(2):
                        yb = bank[6 + n]
                        for k in range(8):
                            K.mm(yb[:, :], catT[:, k * 128:(k + 1) * 128], wout[:, k * 1024 + n * 512:k * 1024 + (n + 1) * 512], k == 0, k == 7, [catT, wout], [yb])
                        cs = slice(n * 512, (n + 1) * 512)
                        K.tt(tmp[:, :], yb[:, :], G1[:, cs], ALU.mult, [yb, G1], [tmp])
                        K.tt(xt[:, cs], tmp[:, :], xt[:, cs], ALU.add, [tmp, xt], [xt])
                        tick()
                    K.dma(XMID0[e].ap, xt[:, :], [xt], [XMID0[e]], xt.sem)

                def conv2():
                    for _ in range(2):
                        if conv_jobs:
                            conv_jobs.pop(0)()

                for e in range(3):
                    conv2()
                    for _ in stageA(e):
                        pass
                for e in range(1, NEXT - 1):
                    conv2()
                    gen = stageA(e + 2) if e + 2 < NEXT else iter(())

                    def tick(gen=gen):
                        next(gen, None)
                        next(gen, None)

                    stageB(e, tick)
                    for _ in gen:
                        pass
                while conv_jobs:
                    conv_jobs.pop(0)()
                P.end_phase()

        def phase_tabconv(l):
            with ExitStack() as st:
                ins = [T(K, st, "cin%d" % i, [128, 8192]) for i in range(2)]
                outs = [T(K, st, "cout%d" % i, [128, 8192], BF16) for i in range(2)]
                for i in range(32):
                    a = ins[i % 2]
                    o = outs[i % 2]
                    K.dma(a[:, :], tab[l][i * 512:(i + 1) * 512, :].rearrange("(p r) c -> p (r c)", r=4), [], [a], a.sem)
                    K.acopy(o[:, 0:3072], a[:, 0:3072], [a], [o])
                    K.cp(o[:, 3072:6144], a[:, 3072:6144], [a], [o])
                    K.cp(o[:, 6144:8192], a[:, 6144:8192], [a], [o], eng="pool")
                    K.dma(tabbf[l][i * 512:(i + 1) * 512, :].rearrange("(p r) c -> p (r c)", r=4), o[:, :], [o], [TABBF[l]], o.sem)
                P.end_phase()

        def phase_peer(l, tiles, src, dst, final):
            with ExitStack() as st:
                SH2 = T(K, st, "SH2", [128, D]); SC2 = T(K, st, "SC2", [128, D]); G2 = T(K, st, "G2", [128, D])
                wq = T(K, st, "wq", [128, 8 * 2048])
                skl = T(K, st, "skl", [128, 256])
                skT = T(K, st, "skT", [128, 256])
                fg = T(K, st, "fg", [128, D]) if final else None
                xts = [T(K, st, "xt%d" % i, [128, D]) for i in range(3)]
                h2s = [T(K, st, "h2_%d" % i, [128, D]) for i in range(2)]
                h2bs = [T(K, st, "h2b_%d" % i, [128, D], BF16) for i in range(2)]
                tmpb = T(K, st, "tmpb", [128, D], BF16)
                ss = T(K, st, "ss", [128, 1]); rstd = T(K, st, "rstd", [128, 1])
                ss2 = T(K, st, "ss2", [128, 1]); rstd2 = T(K, st, "rstd2", [128, 1])
                S0 = T(K, st, "S0", [128, 2048]); S1 = T(K, st, "S1", [128, 2048])
                S2 = T(K, st, "S2", [128, 2048]); S3 = T(K, st, "S3", [128, 2048])
                top = T(K, st, "top", [128, 256]); tidx = T(K, st, "tidx", [128, 256], U32)
                tidxf = T(K, st, "tidxf", [128, 256])
                best = T(K, st, "best", [128, 128]); pos = T(K, st, "pos", [128, 128], U32)
                pa = T(K, st, "pa", [128, 128], U32); pb = T(K, st, "pb", [128, 128], U32)
                sel1 = T(K, st, "sel1", [128, 128]); sel2 = T(K, st, "sel2", [128, 128])
                eidxs = [T(K, st, "eidx%d" % i, [128, 128], U32) for i in range(2)]
                gates = [T(K, st, "gate%d" % i, [128, 128]) for i in range(2)]
                gsum = T(K, st, "gsum", [128, 8])
                dd = T(K, st, "dd", [128, 128]); gd = T(K, st, "gd", [128, 128]); coef = T(K, st, "coef", [128, 128])
                NR = 12
                rows = [T(K, st, "rows%d" % i, [128, 2048], BF16) for i in range(NR)]
                tmp = T(K, st, "tmp", [128, D])
                identb = T(K, st, "identb", [128, 128], BF16)
                diags = [T(K, st, "diag%d" % i, [128, 128], BF16) for i in range(4)]
                K.cp(identb[:, :], ident[:, :], [ident], [identb])
                ddb = [Buf("dd%d" % j) for j in range(128)]
                gdb = [Buf("gd%d" % j) for j in range(128)]

                for (t_, m_) in ((SH2, 3), (SC2, 4), (G2, 5)):
                    K.dma(t_[:, :], MODS[l][m_].ap, [MODS[l][m_]], [t_], t_.sem)
                K.dma(wq[:, :].rearrange("p (k n) -> p k n", n=2048), w_q[l].rearrange("(k p) n -> p k n", p=128), [], [wq], wq.sem)
                K.dma(skl[:, :].rearrange("p (a k) -> p a k", k=128), subk[l].rearrange("a n k -> n a k"), [], [skl], skl.sem)
                if final:
                    K.dma(fg[:, :], final_g.partition_broadcast(128), [], [fg], fg.sem)
                for a in range(2):
                    K.tr(bank[0][:, a * 128:(a + 1) * 128], skl[:, a * 128:(a + 1) * 128], ident[:, :], [skl, ident], [bank[0]])
                K.acopy(skT[:, :], bank[0][:, 0:256], [bank[0]], [skT])

                def routing(ti):
                    e = tiles[ti]
                    xt = xts[ti % 3]
                    h2 = h2s[ti % 2]
                    eidx = eidxs[ti % 2]
                    gate = gates[ti % 2]
                    K.norm_mod(xt, h2, SC2, SH2, S3, ss, rstd)
                    K.acopy(h2bs[ti % 2][:, :], h2[:, :], [h2], [h2bs[ti % 2]])
                    yield
                    h2T = S0
                    K.transpose8(h2, h2T, ident, [bank[0], bank[1]])
                    yield
                    qT = S1
                    for c in range(16):
                        bk = bank[2 + (c // 4) % 2]
                        for k in range(8):
                            K.mm(bk[:, (c % 4) * 128:(c % 4 + 1) * 128], wq[:, k * 2048 + c * 128:k * 2048 + (c + 1) * 128],
                                 h2T[:, k * 128:(k + 1) * 128], k == 0, k == 7, [wq, h2T], [bk])
                            if k % 2 == 1:
                                if k == 7 and c % 4 == 3:
                                    K.acopy(qT[:, (c // 4) * 512:(c // 4 + 1) * 512], bk[:, :], [bk], [qT])
                                yield
                    sc = S0
                    sbanks = [bank[0], bank[1], bank[0], bank[1]]
                    for c in range(16):
                        bk = sbanks[c // 4]
                        K.mm(bk[:, (c % 4) * 128:(c % 4 + 1) * 128], qT[:, c * 128:(c + 1) * 128], skT[:, (c % 2) * 128:(c % 2 + 1) * 128],
                             True, True, [qT, skT], [bk])
                        if c % 4 == 3:
                            K.acopy(sc[:, (c // 4) * 512:(c // 4 + 1) * 512], bk[:, :], [bk], [sc])
                            yield
                    work = S2
                    for c in range(16):
                        cs = slice(c * 128, (c + 1) * 128)
                        t0 = slice(c * 16, c * 16 + 8)
                        t1 = slice(c * 16 + 8, c * 16 + 16)
                        K.vmax(top[:, t0], sc[:, cs], [sc], [top])
                        K.vmaxidx(tidx[:, t0], top[:, t0], sc[:, cs], [sc, top], [tidx])
                        K.vmr(work[:, cs], top[:, t0], sc[:, cs], [sc, top], [work])
                        yield
                        K.vmax(top[:, t1], work[:, cs], [work], [top])
                        K.vmaxidx(tidx[:, t1], top[:, t1], work[:, cs], [work, top], [tidx])
                        yield
                    cand = S1
                    tv = top[:, :].rearrange("p (h two a) -> p h two a", two=2, a=16)
                    K.tt(cand[:, :].rearrange("p (h a b) -> p h a b", a=16, b=16),
                         tv[:, :, 0, :].unsqueeze(3).to_broadcast([128, 8, 16, 16]),
                         tv[:, :, 1, :].unsqueeze(2).to_broadcast([128, 8, 16, 16]), ALU.add, [top], [cand])
                    yield
                    cwork = S2
                    for hh in range(8):
                        cs = slice(hh * 256, (hh + 1) * 256)
                        t0 = slice(hh * 16, hh * 16 + 8)
                        t1 = slice(hh * 16 + 8, hh * 16 + 16)
                        K.vmax(best[:, t0], cand[:, cs], [cand], [best])
                        K.vmaxidx(pos[:, t0], best[:, t0], cand[:, cs], [cand, best], [pos])
                        K.vmr(cwork[:, cs], best[:, t0], cand[:, cs], [cand, best], [cwork])
                        yield
                        K.vmax(best[:, t1], cwork[:, cs], [cwork], [best])
                        K.vmaxidx(pos[:, t1], best[:, t1], cwork[:, cs], [cwork, best], [pos])
                        yield
                    P.op("dve", lambda e_: e_.tensor_single_scalar(out=pa[:, :], in_=pos[:, :], scalar=4, op=ALU.logical_shift_right), [pos], [pa])
                    P.op("dve", lambda e_: e_.tensor_single_scalar(out=pb[:, :], in_=pos[:, :], scalar=15, op=ALU.bitwise_and), [pos], [pb])
                    K.cp(tidxf[:, :], tidx[:, :], [tidx], [tidxf])
                    yield
                    oh = S3
                    tf = tidxf[:, :].rearrange("p (h two a) -> p h two a", two=2, a=16)
                    for which, (pp, sel) in enumerate(((pa, sel1), (pb, sel2))):
                        K.tt(oh[:, :].rearrange("p (h k a) -> p h k a", k=16, a=16),
                             pp[:, :].rearrange("p (h k) -> p h k", k=16).unsqueeze(3).to_broadcast([128, 8, 16, 16]),
                             iota16[:, :].unsqueeze(1).unsqueeze(1).to_broadcast([128, 8, 16, 16]), ALU.is_equal, [pp, iota16], [oh])
                        yield
                        K.tt(oh[:, :].rearrange("p (h k a) -> p h k a", k=16, a=16),
                             oh[:, :].rearrange("p (h k a) -> p h k a", k=16, a=16),
                             tf[:, :, which, :].unsqueeze(2).to_broadcast([128, 8, 16, 16]), ALU.mult, [oh, tidxf], [oh])
                        yield
                        K.red(sel[:, :], oh[:, :].rearrange("p (n a) -> p n a", a=16), ALU.add, [oh], [sel])
                        yield
                    K.stt(sel1[:, :], sel1[:, :], 128.0, sel2[:, :], ALU.mult, ALU.add, [sel1, sel2], [sel1])
                    K.cp(eidx[:, :], sel1[:, :], [sel1], [eidx])
                    b3 = best[:, :].rearrange("p (h k) -> p h k", k=16)
                    K.tt(gate[:, :].rearrange("p (h k) -> p h k", k=16), b3, b3[:, :, 0:1].to_broadcast([128, 8, 16]), ALU.subtract, [best], [gate])
                    K.act(gate[:, :], gate[:, :], AF.Exp, [gate], [gate])
                    K.red(gsum[:, :], gate[:, :].rearrange("p (h k) -> p h k", k=16), ALU.add, [gate], [gsum])
                    K.recip(gsum[:, :], gsum[:, :], [gsum], [gsum])
                    K.tt(gate[:, :].rearrange("p (h k) -> p h k", k=16), gate[:, :].rearrange("p (h k) -> p h k", k=16),
                         gsum[:, :].unsqueeze(2).to_broadcast([128, 8, 16]), ALU.mult, [gate, gsum], [gate])
                    yield

                def load_x(ti):
                    if ti < len(tiles):
                        xt_ = xts[ti % 3]
                        K.dma(xt_[:, :], src[tiles[ti]].ap, [src[tiles[ti]]], [xt_], xt_.sem)

                def accbanks(ti):
                    return [bank[4], bank[5]] if ti % 2 else [bank[6], bank[7]]

                def epilogue(ti):
                    e = tiles[ti]
                    xt = xts[ti % 3]
                    accb = accbanks(ti)
                    for n in range(2):
                        cs = slice(n * 512, (n + 1) * 512)
                        K.tt(tmp[:, cs], accb[n][:, :], G2[:, cs], ALU.mult, [accb[n], G2], [tmp])
                    K.tt(xt[:, :], tmp[:, :], xt[:, :], ALU.add, [tmp, xt], [xt])
                    if not final:
                        K.dma(dst[e].ap, xt[:, :], [xt], [dst[e]], xt.sem)
                    else:
                        K.rstd_of(xt, tmp, ss2, rstd2)
                        K.stt(tmp[:, :], xt[:, :], rstd2[:, 0:1], fg[:, :], ALU.mult, ALU.mult, [xt, rstd2, fg], [tmp])
                        K.dma(dst[e].ap, tmp[:, :], [tmp], [dst[e]], tmp.sem)

                def consume(ti, nxt):
                    e = tiles[ti]
                    h2b = h2bs[ti % 2]
                    eidx = eidxs[ti % 2]
                    gate = gates[ti % 2]
                    accb = accbanks(ti)

                    def fin(j):
                        rw = rows[j % NR]
                        dg = diags[j % 4]
                        K.act(dg[:, :], identb[:, :], AF.Copy, [identb, gdb[j]], [dg], scale=coef[:, j:j + 1])
                        for n in range(2):
                            K.mm(accb[n][:, :], dg[:, :], rw[:, 1024 + n * 512:1024 + (n + 1) * 512], j == 0, j == 127, [dg, rw], [accb[n]])

                    for j in range(128):
                        rw = rows[j % NR]
                        K.gather(rw[:, :], tabbf[l], eidx[:, j:j + 1], [eidx, TABBF[l]], [rw], rw.sem)
                        K.ttr(tmpb[:, :], rw[:, 0:1024], h2b[:, :], dd[:, j:j + 1], [rw, h2b], [tmpb, ddb[j]])
                        K.act(gd[:, j:j + 1], dd[:, j:j + 1], GELU, [ddb[j]], [gdb[j]])
                        K.act(coef[:, j:j + 1], gd[:, j:j + 1], AF.Copy, [gdb[j], gate], [gdb[j]], scale=gate[:, j:j + 1])
                        fin(j)
                        if nxt is not None and j >= 2:
                            if j < 72:
                                npull = 1
                            elif j < 80:
                                npull = 0
                            else:
                                npull = 2 if j % 4 == 1 else 1
                            for _ in range(npull):
                                next(nxt, None)
                        if j == 8:
                            if ti > 0:
                                epilogue(ti - 1)
                            load_x(ti + 2)
                    if nxt is not None:
                        for _ in nxt:
                            pass
                    if ti == len(tiles) - 1:
                        epilogue(ti)

                load_x(0)
                load_x(1)
                for _ in routing(0):
                    pass
                for ti in range(len(tiles)):
                    nxt = routing(ti + 1) if ti + 1 < len(tiles) else None
                    consume(ti, nxt)
                P.end_phase()

        def phase_mixer1():
            with ExitStack() as st:
                SH1 = T(K, st, "SH1", [128, D]); SC1 = T(K, st, "SC1", [128, D]); G1 = T(K, st, "G1", [128, D])
                BT = T(K, st, "BT", [128, 36 * 128])
                wp = T(K, st, "wp", [128, 8 * 256])
                xts = [T(K, st, "xt%d" % i, [128, D]) for i in range(4)]
                hs = [T(K, st, "h%d" % i, [128, D]) for i in range(4)]
                ss = T(K, st, "ss", [128, 1]); rstd = T(K, st, "rstd", [128, 1])
                pT = T(K, st, "pT", [128, D])
                tmp = T(K, st, "tmp", [128, 512])
                for (t_, m_) in ((SH1, 0), (SC1, 1), (G1, 2)):
                    K.dma(t_[:, :], MODS[1][m_].ap, [MODS[1][m_]], [t_], t_.sem)
                K.dma(BT[:, :].rearrange("p (m t) -> p m t", t=128), poolBT, [], [BT], BT.sem)
                K.dma(wp[:, :].rearrange("p (g i e) -> p g i e", g=4, i=2), pool_w.rearrange("g (i c) e -> c g i e", i=2), [], [wp], wp.sem)

                def stA(e):
                    xt = xts[e % 4]
                    K.dma(xt[:, :], X1[e].ap, [X1[e]], [xt], xt.sem)
                    K.norm_mod(xt, hs[e % 4], SC1, SH1, hs[e % 4], ss, rstd)

                pTs = [pT, T(K, st, "pTb", [128, D])]

                def pooled(j):
                    e = j + 2
                    kind = 0 if j == 0 else (2 if j == NOWN - 1 else 1)
                    pbk = [bank[0], bank[1]] if j % 2 == 0 else [bank[4], bank[5]]
                    pt_ = pTs[j % 2]
                    for cc in range(8):
                        g = cc // 2
                        bk = pbk[cc // 4]
                        for s_ in range(3):
                            hsrc = hs[(e - 1 + s_) % 4]
                            m = (kind * 3 + s_) * 4 + g
                            K.mm(bk[:, (cc % 4) * 128:(cc % 4 + 1) * 128], hsrc[:, cc * 128:(cc + 1) * 128], BT[:, m * 128:(m + 1) * 128],
                                 s_ == 0, s_ == 2, [hsrc, BT], [bk])
                        if cc % 4 == 3:
                            K.acopy(pt_[:, (cc // 4) * 512:(cc // 4 + 1) * 512], bk[:, :], [bk], [pt_])

                def rest(j):
                    e = j + 2
                    xt = xts[e % 4]
                    pt_ = pTs[j % 2]
                    ybk = [bank[2], bank[3]] if j % 2 == 0 else [bank[6], bank[7]]
                    for g in range(4):
                        yb = ybk[g // 2]
                        for i in range(2):
                            K.mm(yb[:, (g % 2) * 256:(g % 2 + 1) * 256], pt_[:, (2 * g + i) * 128:(2 * g + i + 1) * 128],
                                 wp[:, (g * 2 + i) * 256:(g * 2 + i + 1) * 256], i == 0, i == 1, [pt_, wp], [yb])
                    for n in range(2):
                        yb = ybk[n]
                        cs = slice(n * 512, (n + 1) * 512)
                        K.tt(tmp[:, :], yb[:, :], G1[:, cs], ALU.mult, [yb, G1], [tmp])
                        K.tt(xt[:, cs], tmp[:, :], xt[:, cs], ALU.add, [tmp, xt], [xt])
                    K.dma(XMID1[j].ap, xt[:, :], [xt], [XMID1[j]], xt.sem)

                for e in range(1, NEXT - 1):
                    stA(e)
                    if e >= 3:
                        pooled(e - 3)
                    if e >= 4:
                        rest(e - 4)
                rest(NOWN - 1)
                P.end_phase()

        conv_jobs = []

        def mk_conv(l, i, cs_):
            return lambda: K.dma(tabbf[l][i * 512:(i + 1) * 512, :], tab[l][i * 512:(i + 1) * 512, :], [], [TABBF[l]], cs_, eng="pool")

        for l in range(2):
            cs_ = Sem(G.enter_context(nc.semaphore("conv%d" % l)), "conv%d" % l)
            for i in range(32):
                conv_jobs.append(mk_conv(l, i, cs_))
        phase_mods(0)
        phase_ctx()
        phase_mixer0()
        phase_peer(0, list(range(1, NEXT - 1)), XMID0, X1, False)
        phase_mods(1)
        phase_mixer1()
        own_src = {j: XMID1[j] for j in range(NOWN)}
        phase_peer(1, list(range(NOWN)), own_src, OUT, True)
    return nc


def _rope_table(pos):
    rowp = (pos // 64).astype(np.float32)
    colp = (pos % 64).astype(np.float32)
    inv = np.power(np.float32(10000.0), -np.arange(16, dtype=np.float32) / np.float32(16)).astype(np.float32)
    ar = rowp[:, None] * inv[None, :]
    ac = colp[:, None] * inv[None, :]
    cr, sr, cc, sc = np.cos(ar), np.sin(ar), np.cos(ac), np.sin(ac)
    cos64 = np.concatenate([cr, cr, cc, cc], axis=1)
    sin64 = np.concatenate([-sr, sr, -sc, sc], axis=1)
    return np.concatenate([cos64, sin64], axis=1).astype(np.float32)


def _pool_BT(base, L):
    out = np.zeros((3, 4, 128, 128), np.float32)
    for g, w in enumerate((2, 4, 8, 16)):
        for t in range(128):
            tp = base + t
            lo = max(tp - w // 2, 0)
            hi = min(tp + w // 2, L)
            cnt = hi - lo
            for src in range(lo, hi):
                rel = src - (base - 128)
                s, r = rel // 128, rel % 128
                out[s, g, r, t] += 1.0 / cnt
            out[1, g, t, t] -= 1.0
    return out


def _prep(inputs):
    f = lambda a: np.ascontiguousarray(np.asarray(a, dtype=np.float32))
    x = f(inputs["x"]); c = f(inputs["c"]); ctx = f(inputs["ctx"]); c_ctx = f(inputs["c_ctx"])
    L = x.shape[1]
    shared = {
        "mod_w": f(inputs["mod_w"]), "mod_b": f(inputs["mod_b"]),
        "norm1_g": f(inputs["norm1_g"]), "norm2_g": f(inputs["norm2_g"]),
        "attn_in_w": f(inputs["attn_in_w"][0]), "attn_out_w": f(inputs["attn_out_w"][0]),
        "attn_sink": f(inputs["attn_sink"][0]), "gmlp_w_s": f(inputs["gmlp_w_s"][0]),
        "bs_col": f(np.asarray(inputs["gmlp_b_s"][0]).T),
        "pool_w": f(inputs["pool_w"][0]), "pool_scale": f(inputs["pool_scale"][0]),
        "peer_w_q": f(inputs["peer_w_q"]), "peer_sub_keys": f(inputs["peer_sub_keys"]),
        "peer_tab0": np.ascontiguousarray(np.concatenate([np.asarray(inputs["peer_down"][0], np.float32), np.asarray(inputs["peer_up"][0], np.float32)], axis=1)),
        "peer_tab1": np.ascontiguousarray(np.concatenate([np.asarray(inputs["peer_down"][1], np.float32), np.asarray(inputs["peer_up"][1], np.float32)], axis=1)),
        "final_g": f(inputs["final_g"]),
        "ident": np.eye(128, dtype=np.float32),
        "iota16": np.tile(np.arange(16, dtype=np.float32)[None, :], (128, 1)),
    }
    jj = np.arange(128)[:, None]
    ii = np.arange(128)[None, :]
    masks = np.stack([(ii <= jj), (jj <= ii)], axis=1).astype(np.float32)
    shared["masks"] = np.ascontiguousarray(masks)
    maps = []
    for core in range(NCORE):
        b, q = core // 4, core % 4
        start = q * OWN
        lo = start - 256
        xe = np.zeros((NEXT * 128, D), np.float32)
        s0, s1 = max(lo, 0), min(lo + NEXT * 128, L)
        xe[s0 - lo:s1 - lo] = x[b, s0:s1]
        pos = np.clip(np.arange(lo, lo + NEXT * 128), 0, L - 1)
        kv = np.array([1.0 if 0 <= lo + e * 128 < L else 0.0 for e in range(NEXT)], np.float32)
        BT = np.zeros((3, 3, 4, 128, 128), np.float32)
        for kind, j in enumerate((0, 1, NOWN - 1)):
            BT[kind] = _pool_BT(start + j * 128, L)
        BTl = np.ascontiguousarray(BT.reshape(36, 128, 128).transpose(1, 0, 2))
        m = dict(shared)
        m.update({
            "x_ext": xe,
            "ctx_b": np.ascontiguousarray(ctx[b]),
            "c_cols": np.ascontiguousarray(np.concatenate([c[b].reshape(8, 128).T, c_ctx.reshape(8, 128).T], axis=1)),
            "rope": _rope_table(pos),
            "kvalid": np.ascontiguousarray(np.tile(kv[None, :], (128, 1))),
            "poolBT": BTl,
        })
        maps.append(m)
    return maps


_NC_CACHE = {}


def kernel(**inputs):
    maps = _prep(inputs)
    if "nc" not in _NC_CACHE:
        _NC_CACHE["nc"] = build_nc(False)
    nc = _NC_CACHE["nc"]
    res = run_bass_kernel_spmd(nc, maps, core_ids=list(range(NCORE)))
    B = 2
    out = np.zeros((B, 4 * OWN, D), np.float32)
    for core in range(NCORE):
        b, q = core // 4, core % 4
        out[b, q * OWN:(q + 1) * OWN] = np.asarray(res.results[core]["out"], np.float32)
    return out
```
